# Optimizing a Trainium2 kernel written in Bass

```python
import math
import jax, jax.numpy as jnp
from jax import lax
import numpy as np

D_MODEL = 4096
BATCH = 1
SEQ = 8192
DEPTH = 4

HEAD_DIM = 128
POOL_WINDOWS = (2, 4, 8, 16)
POOL_GROUPS = len(POOL_WINDOWS)
POOL_WIDTH = D_MODEL // 4
POOL_GROUP_DIM = POOL_WIDTH // POOL_GROUPS
DSA_WIDTH = 3 * D_MODEL // 8
DSA_HEADS = DSA_WIDTH // HEAD_DIM
SB_WIDTH = D_MODEL - POOL_WIDTH - DSA_WIDTH
SB_HEADS = SB_WIDTH // HEAD_DIM
MIX_WIDTH = POOL_WIDTH + DSA_WIDTH + SB_WIDTH
IDX_HEADS = 16
IDX_DIM = 64
INDEX_TOPK = 256
D_FF = 3 * D_MODEL // 2
N_BUCKETS = 32
MAX_DISTANCE = 128
Q_BLOCK = 128
EPS = 1e-6
IN_SIZES = (POOL_WIDTH, DSA_WIDTH, DSA_WIDTH, DSA_WIDTH,
            IDX_HEADS * IDX_DIM, IDX_DIM, IDX_HEADS,
            SB_WIDTH, SB_WIDTH, SB_WIDTH)
IN_WIDTH = sum(IN_SIZES)

kernel_name = "hymba_style_pool_dsa_stickbreak_macaron"


def rmsnorm(x, g):
    xf = x.astype(jnp.float32)
    y = xf * lax.rsqrt(jnp.mean(xf * xf, axis=-1, keepdims=True) + EPS)
    return (y * g.astype(jnp.float32)).astype(x.dtype)


def swiglu(x, wg, wu, wd):
    return (jax.nn.silu(x @ wg) * (x @ wu)) @ wd


def rel_bucket(n):
    n = jnp.maximum(n, 0)
    max_exact = N_BUCKETS // 2
    nf = jnp.maximum(n, 1).astype(jnp.float32)
    large = max_exact + (jnp.log(nf / max_exact) / math.log(MAX_DISTANCE / max_exact)
                         * (N_BUCKETS - max_exact)).astype(jnp.int32)
    large = jnp.minimum(large, N_BUCKETS - 1)
    return jnp.where(n < max_exact, n, large)


def to_blocks(t):
    b, s = t.shape[0], t.shape[1]
    return jnp.moveaxis(t.reshape((b, s // Q_BLOCK, Q_BLOCK) + t.shape[2:]), 1, 0)


def from_blocks(t):
    t = jnp.moveaxis(t, 0, 1)
    return t.reshape((t.shape[0], t.shape[1] * t.shape[2]) + t.shape[3:])


def pool_mixer(u, pool_w, pool_scale):
    b, s, _ = u.shape
    uf = u.astype(jnp.float32)
    c = lax.cumsum(uf, axis=1)
    c0 = jnp.concatenate([jnp.zeros_like(c[:, :1]), c[:, :-1]], axis=1)
    t = jnp.arange(s)
    outs = []
    for g, w in enumerate(POOL_WINDOWS):
        sl = slice(g * POOL_GROUP_DIM, (g + 1) * POOL_GROUP_DIM)
        lag = jnp.concatenate([jnp.zeros((b, w - 1, POOL_GROUP_DIM), jnp.float32),
                               c0[:, :s - w + 1, sl]], axis=1)
        cnt = jnp.minimum(t + 1, w).astype(jnp.float32)[None, :, None]
        d = (c[..., sl] - lag) / cnt - uf[..., sl]
        outs.append(d.astype(u.dtype) @ pool_w[g])
    return jnp.concatenate(outs, axis=-1) * pool_scale


def dsa_mixer(q, k, v, qi, ki, wi, pos, rel_bias, topk):
    def block(args):
        qb, qib, wib, pb = args
        sc = jnp.einsum('bqhd,bsd->bqhs', qib, ki) * (IDX_DIM ** -0.5)
        idx_score = jnp.einsum('bqhs,bqh->bqs', jax.nn.relu(sc), wib * (IDX_HEADS ** -0.5))
        admiss = pos[:, None, :] <= pb[:, :, None]
        idx_score = jnp.where(admiss, idx_score.astype(jnp.float32), -jnp.inf)
        _, sel = lax.top_k(idx_score, topk)
        sel_pos = jax.vmap(lambda p, ii: p[ii])(pos, sel)
        ks = jax.vmap(lambda kk, ii: kk[ii])(k, sel)
        vs = jax.vmap(lambda vv, ii: vv[ii])(v, sel)
        valid = sel_pos <= pb[:, :, None]
        logits = jnp.einsum('bqhd,bqkhd->bhqk', qb, ks).astype(jnp.float32) * (HEAD_DIM ** -0.5)
        bias = jnp.transpose(rel_bias[rel_bucket(pb[:, :, None] - sel_pos)], (0, 3, 1, 2))
        logits = jnp.where(valid[:, None], logits + bias.astype(jnp.float32), -jnp.inf)
        p = jax.nn.softmax(logits, axis=-1)
        return jnp.einsum('bhqk,bqkhd->bqhd', p.astype(v.dtype), vs)

    out = lax.map(block, (to_blocks(q), to_blocks(qi), to_blocks(wi), to_blocks(pos)))
    return from_blocks(out)


def stickbreak_mixer(q, k, v, pos):
    def block(args):
        qb, pb = args
        z = jnp.einsum('bqhd,bshd->bhqs', qb, k).astype(jnp.float32) * (HEAD_DIM ** -0.5)
        strict = (pos[:, None, :] < pb[:, :, None])[:, None]
        log_1m = jnp.where(strict, jax.nn.log_sigmoid(-z), 0.0)
        later = lax.cumsum(log_1m, axis=3, reverse=True) - log_1m
        a = jnp.where(strict, jnp.exp(jax.nn.log_sigmoid(z) + later), 0.0)
        return jnp.einsum('bhqs,bshd->bqhd', a.astype(v.dtype), v)

    out = lax.map(block, (to_blocks(q), to_blocks(pos)))
    return from_blocks(out)


def setup_inputs(seed: int = 0) -> dict:
    key = jax.random.key(seed)
    ks = jax.random.split(key, 20)
    f32 = jnp.float32
    nrm = lambda k, shape, scale: jax.random.normal(k, shape, f32) * scale
    gain = lambda k, shape: 1.0 + 0.02 * jax.random.normal(k, shape, f32)
    return {
        "x": jax.random.normal(ks[0], (BATCH, SEQ, D_MODEL), f32),
        "positions": jnp.broadcast_to(jnp.arange(SEQ, dtype=jnp.int32), (BATCH, SEQ)),
        "rel_bias": nrm(ks[1], (N_BUCKETS, DSA_HEADS), 0.5),
        "ffn1_norm": gain(ks[2], (DEPTH, D_MODEL)),
        "ffn1_gate": nrm(ks[3], (DEPTH, D_MODEL, D_FF), D_MODEL ** -0.5),
        "ffn1_up": nrm(ks[4], (DEPTH, D_MODEL, D_FF), D_MODEL ** -0.5),
        "ffn1_down": nrm(ks[5], (DEPTH, D_FF, D_MODEL), D_FF ** -0.5),
        "mix_norm": gain(ks[6], (DEPTH, D_MODEL)),
        "w_in": nrm(ks[7], (DEPTH, D_MODEL, IN_WIDTH), D_MODEL ** -0.5),
        "pool_w": nrm(ks[8], (DEPTH, POOL_GROUPS, POOL_GROUP_DIM, POOL_GROUP_DIM), POOL_GROUP_DIM ** -0.5),
        "pool_scale": 1.0 + 0.1 * jax.random.normal(ks[9], (DEPTH, POOL_WIDTH), f32),
        "q_norm": gain(ks[10], (DEPTH, HEAD_DIM)),
        "k_norm": gain(ks[11], (DEPTH, HEAD_DIM)),
        "w_out": nrm(ks[12], (DEPTH, MIX_WIDTH, D_MODEL), MIX_WIDTH ** -0.5),
        "ffn2_norm": gain(ks[13], (DEPTH, D_MODEL)),
        "ffn2_gate": nrm(ks[14], (DEPTH, D_MODEL, D_FF), D_MODEL ** -0.5),
        "ffn2_up": nrm(ks[15], (DEPTH, D_MODEL, D_FF), D_MODEL ** -0.5),
        "ffn2_down": nrm(ks[16], (DEPTH, D_FF, D_MODEL), D_FF ** -0.5),
    }


def reference(x, positions, rel_bias, ffn1_norm, ffn1_gate, ffn1_up, ffn1_down,
              mix_norm, w_in, pool_w, pool_scale, q_norm, k_norm, w_out,
              ffn2_norm, ffn2_gate, ffn2_up, ffn2_down):
    b, s, _ = x.shape
    topk = min(INDEX_TOPK, s // 4)
    offsets = [int(o) for o in np.cumsum(IN_SIZES)[:-1]]
    for i in range(DEPTH):
        h = x + 0.5 * swiglu(rmsnorm(x, ffn1_norm[i]), ffn1_gate[i], ffn1_up[i], ffn1_down[i])
        u = rmsnorm(h, mix_norm[i]) @ w_in[i]
        (u_pool, qb, kb, vb, qi, ki, wi, qc, kc, vc) = jnp.split(u, offsets, axis=-1)
        y_pool = pool_mixer(u_pool, pool_w[i], pool_scale[i])
        qb = rmsnorm(qb.reshape(b, s, DSA_HEADS, HEAD_DIM), q_norm[i])
        kb = rmsnorm(kb.reshape(b, s, DSA_HEADS, HEAD_DIM), k_norm[i])
        vb = vb.reshape(b, s, DSA_HEADS, HEAD_DIM)
        y_dsa = dsa_mixer(qb, kb, vb, qi.reshape(b, s, IDX_HEADS, IDX_DIM), ki, wi,
                          positions, rel_bias, topk).reshape(b, s, DSA_WIDTH)
        y_sb = stickbreak_mixer(qc.reshape(b, s, SB_HEADS, HEAD_DIM),
                                kc.reshape(b, s, SB_HEADS, HEAD_DIM),
                                vc.reshape(b, s, SB_HEADS, HEAD_DIM),
                                positions).reshape(b, s, SB_WIDTH)
        h = h + jnp.concatenate([y_pool, y_dsa, y_sb], axis=-1) @ w_out[i]
        x = h + 0.5 * swiglu(rmsnorm(h, ffn2_norm[i]), ffn2_gate[i], ffn2_up[i], ffn2_down[i])
    return x
```

```python
import math
import time
from contextlib import ExitStack
import numpy as np
import ml_dtypes
import concourse.bass as bass
import concourse.mybir as mybir
from concourse.bass_utils import run_bass_kernel_spmd


F32 = mybir.dt.float32
BF16 = mybir.dt.bfloat16
AF = mybir.ActivationFunctionType
ALU = mybir.AluOpType
AX = mybir.AxisListType

SEM_ROLL = 30000


class Sem:
    def __init__(self, M, name):
        self.M, self.name = M, name
        self.h = M.sem_es.enter_context(M.nc.semaphore(name))
        self.count = 0


class Buf:
    def __init__(self, name, ap=None):
        self.name, self.ap = name, ap
        self.writer = None
        self.readers = []

    def __getitem__(self, idx):
        return self.ap[idx]


class Eng:
    def __init__(self, M, name, e, is_pe=False):
        self.M, self.name, self.e, self.is_pe = M, name, e, is_pe
        self.nsem = 0
        self.sem = None
        self.waited = {}
        self.ndma = 0
        self.dsems = []
        self._roll()

    def _roll(self):
        self.sem = Sem(self.M, f"s_{self.name}_{self.nsem}")
        self.nsem += 1

    def _wait(self, deps):
        best = {}
        for tok in deps:
            if tok is None:
                continue
            s, v = tok
            if best.get(s, 0) < v:
                best[s] = v
        for s, v in best.items():
            if self.is_pe and s is self.sem:
                continue
            if self.waited.get(s, 0) >= v:
                continue
            self.e.wait_ge(s.h, v)
            self.waited[s] = v

    def _deps(self, reads, writes):
        deps = []
        for b in reads:
            deps.append(b.writer)
            if getattr(b, "psum", False):
                deps.extend(t for t in b.readers if t[0] is not self.sem)
        for b in writes:
            deps.append(b.writer)
            deps.extend(b.readers)
        return deps

    def op(self, fn, reads=(), writes=()):
        self._wait(self._deps(reads, writes))
        if self.sem.count >= SEM_ROLL:
            self._roll()
        inst = fn()
        self.sem.count += 1
        inst.then_inc(self.sem.h, 1)
        tok = (self.sem, self.sem.count)
        for b in reads:
            b.readers.append(tok)
            if len(b.readers) > 64:
                b.readers = b.readers[-64:] if False else _compact(b.readers)
        for b in writes:
            b.writer = tok
            b.readers = []
        return tok

    def dma(self, out, in_, reads=(), writes=(), nslots=8, **kw):
        if not self.dsems:
            self.dsems = [Sem(self.M, f"d_{self.name}_{i}") for i in range(nslots)]
        s = self.dsems[self.ndma % len(self.dsems)]
        self.ndma += 1
        deps = self._deps(reads, writes)
        if s.count:
            deps.append((s, s.count))
        self._wait(deps)
        inst = self.e.dma_start(out=out, in_=in_, **kw)
        s.count += 16
        inst.then_inc(s.h, 16)
        tok = (s, s.count)
        for b in reads:
            b.readers.append(tok)
        for b in writes:
            b.writer = tok
            b.readers = []
        return tok

    def wait_tok(self, tok):
        self._wait([tok])


def _compact(readers):
    best = {}
    for s, v in readers:
        if best.get(s, 0) < v:
            best[s] = v
    return list(best.items())


class MK:
    def __init__(self):
        self.nc = bass.Bass("TRN2", target_bir_lowering=False)
        self.es = ExitStack()
        self.sem_es = ExitStack()
        nc = self.nc
        self.pe = Eng(self, "pe", nc.tensor, is_pe=True)
        self.act = Eng(self, "act", nc.scalar)
        self.dve = Eng(self, "dve", nc.vector)
        self.pool = Eng(self, "pool", nc.gpsimd)
        self.sp = Eng(self, "sp", nc.sync)
        self.nbank = 0

    def din(self, name, shape, dt=F32):
        return self.nc.dram_tensor(name, list(shape), dt, kind="ExternalInput").ap()

    def dout(self, name, shape, dt=F32):
        return self.nc.dram_tensor(name, list(shape), dt, kind="ExternalOutput").ap()

    def dscratch(self, name, shape, dt=F32):
        return self.nc.dram_tensor(name, list(shape), dt).ap()

    def sb(self, name, shape, dt=F32):
        t = self.es.enter_context(self.nc.sbuf_tensor(name, list(shape), dt))
        return Buf(name, t)

    def ps(self, name, shape, dt=F32):
        t = self.es.enter_context(self.nc.psum_tensor(name, list(shape), dt))
        b = Buf(name, t); b.psum = True
        return b

    def engines(self):
        return [self.pe, self.act, self.dve, self.pool, self.sp]

    def barrier(self):
        toks = []
        for X in self.engines():
            toks.append((X.sem, X.sem.count))
            toks.extend((d, d.count) for d in X.dsems if d.count)
        toks = [t for t in toks if t[1] > 0]
        for X in self.engines():
            X._wait(toks)

    def phase(self):
        M = self
        class _Ph:
            def __enter__(s):
                s.old = M.es; M.es = ExitStack(); return s
            def __exit__(s, *a):
                M.barrier(); M.es.close(); M.es = s.old; return False
        return _Ph()

    def finish(self, toks):
        self.sp._wait(toks)
        self.es.close()
        self.sem_es.close()
        return self.nc


P = 128
TT = 512


def load_consts(M, C):
    C["ones_bf"] = M.sb("ones_bf", [P, P], BF16)
    M.dve.op(lambda: M.nc.vector.memset(C["ones_bf"][:], 1.0), writes=[C["ones_bf"]])
    C["eps"] = M.sb("eps", [P, 1], F32)
    M.dve.op(lambda: M.nc.vector.memset(C["eps"][:], 1e-6), writes=[C["eps"]])


def rms_rstd(M, C, W, src_chunk_loader, nchunks, D, tag):
    nc = M.nc
    ss = W["ps_ss"]
    for k in range(nchunks):
        xk = src_chunk_loader(k)
        sq = W["sq"][k % 2]
        M.act.op(lambda: nc.scalar.activation(out=sq[:], in_=xk[:], func=AF.Square), reads=[xk], writes=[sq])
        M.pe.op(lambda: nc.tensor.matmul(ss[:], C["ones_bf"][:], sq[:], start=(k == 0), stop=(k == nchunks - 1)),
                reads=[sq, C["ones_bf"]], writes=[ss])
    rstd = W["rstd"]
    M.act.op(lambda: nc.scalar.activation(out=rstd[:], in_=ss[:], func=AF.Sqrt, bias=C["eps"][:], scale=1.0 / D),
             reads=[ss, C["eps"]], writes=[rstd])
    M.dve.op(lambda: nc.vector.reciprocal(out=rstd[:], in_=rstd[:]), reads=[rstd], writes=[rstd])
    return rstd


def make_ffn_work(M, D, F):
    KD, KF = D // P, F // P
    W = {}
    W["xk"] = [M.sb(f"f_xk{i}", [P, TT], F32) for i in range(3)]
    W["sq"] = [M.sb(f"f_sq{i}", [P, TT], BF16) for i in range(2)]
    W["rstd"] = M.sb("f_rstd", [P, TT], F32)
    W["xn"] = M.sb("f_xn", [P, KD, TT], BF16)
    W["h"] = M.sb("f_h", [P, KF * TT], BF16)
    W["wg"] = [M.sb(f"f_wg{i}", [P, KD, P], BF16) for i in range(2)]
    W["wu"] = [M.sb(f"f_wu{i}", [P, KD, P], BF16) for i in range(2)]
    W["wd"] = [M.sb(f"f_wd{i}", [P, KF, P], BF16) for i in range(2)]
    W["sg"] = [M.sb(f"f_sg{i}", [P, TT], F32) for i in range(2)]
    W["yo"] = [M.sb(f"f_yo{i}", [P, TT], F32) for i in range(2)]
    W["ps_ss"] = M.ps("ps_ss", [P, TT], F32)
    W["ps_g"] = [M.ps(f"ps_g{i}", [P, TT], F32) for i in range(2)]
    W["ps_u"] = [M.ps(f"ps_u{i}", [P, TT], F32) for i in range(2)]
    W["ps_y"] = [M.ps(f"ps_y{i}", [P, TT], F32) for i in range(2)]
    return W


def norm_tile(M, C, W, xT, gain_sb, D, t0, xn, c0=0):
    nc = M.nc
    KD = D // P
    def loader(k):
        b = W["xk"][k % 3]
        M.sp.dma(b[:], xT[k * P:(k + 1) * P, t0:t0 + TT], writes=[b])
        return b
    rstd = rms_rstd(M, C, W, loader, KD, D, "n")
    for k in range(KD):
        b = loader(k)
        M.dve.op(lambda: nc.vector.scalar_tensor_tensor(out=xn[:, k, c0:c0 + TT], in0=b[:], scalar=gain_sb[:, k:k + 1], in1=rstd[:],
                                                          op0=ALU.mult, op1=ALU.mult),
                 reads=[b, gain_sb, rstd], writes=[xn])


def ffn_tile(M, C, W, xT, gain_sb, wg, wu, wd, outT, D, F, t0):
    nc = M.nc
    KD, KF = D // P, F // P
    xn, h = W["xn"], W["h"]
    norm_tile(M, C, W, xT, gain_sb, D, t0, xn)
    for j in range(KF):
        bg, bu = W["wg"][j % 2], W["wu"][j % 2]
        M.pool.dma(bg[:], wg[:, j * P:(j + 1) * P].rearrange("(k p) f -> p k f", p=P), writes=[bg])
        M.pool.dma(bu[:], wu[:, j * P:(j + 1) * P].rearrange("(k p) f -> p k f", p=P), writes=[bu])
        pg, pu = W["ps_g"][j % 2], W["ps_u"][j % 2]
        for k in range(KD):
            M.pe.op(lambda: nc.tensor.matmul(pg[:], bg[:, k, :], xn[:, k, :], start=(k == 0), stop=(k == KD - 1)),
                    reads=[bg, xn], writes=[pg])
        for k in range(KD):
            M.pe.op(lambda: nc.tensor.matmul(pu[:], bu[:, k, :], xn[:, k, :], start=(k == 0), stop=(k == KD - 1)),
                    reads=[bu, xn], writes=[pu])
        sg = W["sg"][j % 2]
        M.act.op(lambda: nc.scalar.activation(out=sg[:], in_=pg[:], func=AF.Silu), reads=[pg], writes=[sg])
        M.dve.op(lambda: nc.vector.tensor_tensor(out=h[:, j * TT:(j + 1) * TT], in0=sg[:], in1=pu[:], op=ALU.mult),
                 reads=[sg, pu], writes=[h])
    toks = []
    for i in range(KD):
        bd = W["wd"][i % 2]
        M.pool.dma(bd[:], wd[:, i * P:(i + 1) * P].rearrange("(j p) c -> p j c", p=P), writes=[bd])
        xk = W["xk"][i % 3]
        M.sp.dma(xk[:], xT[i * P:(i + 1) * P, t0:t0 + TT], writes=[xk])
        py = W["ps_y"][i % 2]
        for j in range(KF):
            M.pe.op(lambda: nc.tensor.matmul(py[:], bd[:, j, :], h[:, j * TT:(j + 1) * TT], start=(j == 0), stop=(j == KF - 1)),
                    reads=[bd, h], writes=[py])
        yo = W["yo"][i % 2]
        M.dve.op(lambda: nc.vector.scalar_tensor_tensor(out=yo[:], in0=py[:], scalar=0.5, in1=xk[:], op0=ALU.mult, op1=ALU.add),
                 reads=[py, xk], writes=[yo])
        toks.append(M.sp.dma(outT[i * P:(i + 1) * P, t0:t0 + TT], yo[:], reads=[yo]))
    return toks


def make_ffn_work2(M, D, F, T):
    KD, KF = D // P, F // P
    KH = KF // 2
    W = {}
    W["xk"] = [M.sb(f"f_xk{i}", [P, TT], F32) for i in range(3)]
    W["sq"] = [M.sb(f"f_sq{i}", [P, TT], BF16) for i in range(2)]
    W["rstd"] = M.sb("f_rstd", [P, TT], F32)
    W["xn"] = M.sb("f_xn", [P, KD, T], BF16)
    W["h"] = M.sb("f_h", [P, KH * T], BF16)
    W["wg"] = [M.sb(f"f_wg{i}", [P, KD, P], BF16) for i in range(2)]
    W["wu"] = [M.sb(f"f_wu{i}", [P, KD, P], BF16) for i in range(2)]
    W["wd"] = [M.sb(f"f_wd{i}", [P, KH, P], BF16) for i in range(2)]
    W["sg"] = [M.sb(f"f_sg{i}", [P, TT], F32) for i in range(2)]
    W["yo"] = [M.sb(f"f_yo{i}", [P, TT], F32) for i in range(2)]
    W["pb"] = [M.ps(f"f_pb{i}", [P, TT], F32) for i in range(8)]
    W["ps_ss"] = W["pb"][7]
    return W


def ffn_full(M, C, W, xT, gain_sb, wg, wu, wd, outT, D, F, T):
    nc = M.nc
    KD, KF = D // P, F // P
    KH = KF // 2
    NT = T // TT
    xn, h, PB = W["xn"], W["h"], W["pb"]
    for th in range(NT):
        norm_tile(M, C, W, xT, gain_sb, D, th * TT, xn, c0=th * TT)
    part = {}
    toks = []
    n_ev = 0
    n_dn = 0
    for half in range(2):
        for jj in range(KH):
            j = half * KH + jj
            bg, bu = W["wg"][jj % 2], W["wu"][jj % 2]
            M.pool.dma(bg[:], wg[:, j * P:(j + 1) * P].rearrange("(k p) f -> p k f", p=P), writes=[bg])
            M.pool.dma(bu[:], wu[:, j * P:(j + 1) * P].rearrange("(k p) f -> p k f", p=P), writes=[bu])
            for th in range(NT):
                pg = PB[(jj % 2) * 4 + (th % 2) * 2]
                pu = PB[(jj % 2) * 4 + (th % 2) * 2 + 1]
                tc = slice(th * TT, (th + 1) * TT)
                for k in range(KD):
                    M.pe.op(lambda: nc.tensor.matmul(pg[:], bg[:, k, :], xn[:, k, tc], start=(k == 0), stop=(k == KD - 1)),
                            reads=[bg, xn], writes=[pg])
                for k in range(KD):
                    M.pe.op(lambda: nc.tensor.matmul(pu[:], bu[:, k, :], xn[:, k, tc], start=(k == 0), stop=(k == KD - 1)),
                            reads=[bu, xn], writes=[pu])
                sg = W["sg"][n_ev % 2]; n_ev += 1
                M.act.op(lambda: nc.scalar.activation(out=sg[:], in_=pg[:], func=AF.Silu), reads=[pg], writes=[sg])
                hc = slice(jj * T + th * TT, jj * T + (th + 1) * TT)
                M.dve.op(lambda: nc.vector.tensor_tensor(out=h[:, hc], in0=sg[:], in1=pu[:], op=ALU.mult), reads=[sg, pu], writes=[h])
        its = [(i, th) for i in range(KD) for th in range(NT)]
        def load_x(n):
            i, th = its[n]
            tc = slice(th * TT, (th + 1) * TT)
            xk = W["xk"][(n_dn0 + n) % 3]
            if half == 0:
                M.sp.dma(xk[:], xT[i * P:(i + 1) * P, tc], writes=[xk])
            else:
                M.sp._wait([part[(i, th)]])
                M.sp.dma(xk[:], outT[i * P:(i + 1) * P, tc], writes=[xk])
            return xk
        n_dn0 = n_dn
        xks = {0: load_x(0)}
        for n, (i, th) in enumerate(its):
            if th == 0:
                bd = W["wd"][i % 2]
                M.pool.dma(bd[:], wd[half * KH * P:(half + 1) * KH * P, i * P:(i + 1) * P].rearrange("(j p) c -> p j c", p=P), writes=[bd])
            if n + 1 < len(its):
                xks[n + 1] = load_x(n + 1)
            tc = slice(th * TT, (th + 1) * TT)
            xk = xks.pop(n)
            py = PB[n_dn % 4]
            for jj in range(KH):
                hc = slice(jj * T + th * TT, jj * T + (th + 1) * TT)
                M.pe.op(lambda: nc.tensor.matmul(py[:], bd[:, jj, :], h[:, hc], start=(jj == 0), stop=(jj == KH - 1)),
                        reads=[bd, h], writes=[py])
            yo = W["yo"][n_dn % 2]
            M.dve.op(lambda: nc.vector.scalar_tensor_tensor(out=yo[:], in0=py[:], scalar=0.5, in1=xk[:], op0=ALU.mult, op1=ALU.add),
                     reads=[py, xk], writes=[yo])
            tok = M.sp.dma(outT[i * P:(i + 1) * P, tc], yo[:], reads=[yo])
            if half == 0:
                part[(i, th)] = tok
            else:
                toks.append(tok)
            n_dn += 1
    return toks


POOLW, DSAW, SBW, IDXH, IDXD = 1024, 1536, 1536, 16, 64
HD = 128


def col_offsets(D):
    sizes = [("up", POOLW), ("qb", DSAW), ("kb", DSAW), ("vb", DSAW), ("qi", IDXH * IDXD), ("ki", IDXD), ("wi", IDXH),
             ("qc", SBW), ("kc", SBW), ("vc", SBW)]
    off, o = {}, 0
    for n, s in sizes:
        off[n] = (o, s); o += s
    return off, o


def build_A(D, F, T, sizes=None, feat=("up", "qb", "kb", "qi", "ki", "qc", "kc"), tokm=("vb", "vc", "wi")):
    M = MK(); nc = M.nc
    KD = D // P
    off, INW = col_offsets(D)
    if sizes:
        off, INW = sizes
    xT = M.din("xT", [D, T])
    gains = M.din("gains", [P, 2 * KD])
    qkg = M.din("qkg", [P, 2])
    wg = M.din("wg", [D, F]); wu = M.din("wu", [D, F]); wd = M.din("wd", [F, D])
    w_in = M.din("w_in", [D, INW])
    hT = M.dout("hT", [D, T])
    outs = {}
    for n in ("up", "qb", "kb", "qi", "ki", "qc", "kc"):
        outs[n] = M.dout(n + "T", [off[n][1], T], BF16)
    for n in ("vb", "vc"):
        outs[n] = M.dout(n, [T, off[n][1]], BF16)
    outs["wi"] = M.dout("wi", [T, off["wi"][1]], F32)

    C = {}; load_consts(M, C)
    W = make_ffn_work2(M, D, F, T)
    NT = T // TT
    gs = M.sb("gains_sb", [P, 2 * KD], F32)
    M.sp.dma(gs[:], gains[:, :], writes=[gs])
    qk = M.sb("qkg_sb", [P, 2], F32)
    M.sp.dma(qk[:], qkg[:, :], writes=[qk])
    ev = [M.sb(f"a_ev{i}", [P, TT], BF16) for i in range(2)]
    qf = [M.sb(f"a_qf{i}", [P, TT], F32) for i in range(2)]
    evw = [M.sb(f"a_evw{i}", [P, 16], F32) for i in range(2)]
    PB = W["pb"]
    toks = []
    ftoks = ffn_full(M, C, W, xT, _view(gs, 0, KD), wg, wu, wd, hT, D, F, T)
    toks += ftoks
    M.sp._wait(ftoks)
    g_mix = _view(gs, KD, KD)
    xn = W["xn"]
    for th in range(NT):
        norm_tile(M, C, W, hT, g_mix, D, th * TT, xn, c0=th * TT)
    n_ev = 0
    n_w = 0
    for name in feat:
        o, sz = off[name]
        for c0 in range(0, sz, P):
            m = min(P, sz - c0)
            bw = W["wg"][n_w % 2]; n_w += 1
            M.pool.dma(bw[:, :, 0:m], w_in[:, o + c0:o + c0 + m].rearrange("(k p) f -> p k f", p=P), writes=[bw])
            for th in range(NT):
                tc = slice(th * TT, (th + 1) * TT)
                pp = PB[n_ev % 4]
                for k in range(KD):
                    M.pe.op(lambda: nc.tensor.matmul(pp[0:m, :], bw[:, k, 0:m], xn[:, k, tc], start=(k == 0), stop=(k == KD - 1)),
                            reads=[bw, xn], writes=[pp])
                e = ev[n_ev % 2]
                if name in ("qb", "kb"):
                    gi = 0 if name == "qb" else 1
                    f = qf[n_ev % 2]; sq = W["sq"][n_ev % 2]; ss = PB[4 + n_ev % 2]; rstd = W["rstd"]
                    M.dve.op(lambda: nc.vector.tensor_copy(out=f[:], in_=pp[:]), reads=[pp], writes=[f])
                    M.act.op(lambda: nc.scalar.activation(out=sq[:], in_=f[:], func=AF.Square), reads=[f], writes=[sq])
                    M.pe.op(lambda: nc.tensor.matmul(ss[:], C["ones_bf"][:], sq[:], start=True, stop=True),
                            reads=[sq, C["ones_bf"]], writes=[ss])
                    M.act.op(lambda: nc.scalar.activation(out=rstd[:], in_=ss[:], func=AF.Sqrt, bias=C["eps"][:], scale=1.0 / HD),
                             reads=[ss, C["eps"]], writes=[rstd])
                    M.dve.op(lambda: nc.vector.reciprocal(out=rstd[:], in_=rstd[:]), reads=[rstd], writes=[rstd])
                    M.dve.op(lambda: nc.vector.scalar_tensor_tensor(out=e[:], in0=f[:], scalar=qk[:, gi:gi + 1], in1=rstd[:],
                                                                      op0=ALU.mult, op1=ALU.mult),
                             reads=[f, qk, rstd], writes=[e])
                else:
                    M.act.op(lambda: nc.scalar.copy(out=e[0:m, :], in_=pp[0:m, :]), reads=[pp], writes=[e])
                toks.append(M.sp.dma(outs[name][c0:c0 + m, tc], e[0:m, :], reads=[e]))
                n_ev += 1
    h = W["h"]
    CW = 256
    nv = 0
    n_t = 0
    for name in tokm:
        o, sz = off[name]
        for c0 in range(0, sz, CW):
            m = min(CW, sz - c0)
            base = (nv % 2) * KD * CW
            wv = h.ap[:, base:base + KD * CW].rearrange("p (k c) -> p k c", k=KD)
            M.pool.dma(wv[:, :, 0:m], w_in[:, o + c0:o + c0 + m].rearrange("(k p) f -> p k f", p=P), writes=[h])
            for ts in range(T // P):
                pp = PB[n_t % 4]
                for k in range(KD):
                    M.pe.op(lambda: nc.tensor.matmul(pp[:, 0:m], xn[:, k, ts * P:(ts + 1) * P], wv[:, k, 0:m],
                                                     start=(k == 0), stop=(k == KD - 1)),
                            reads=[h, xn], writes=[pp])
                e = evw[n_t % 2] if name == "wi" else ev[n_t % 2]
                M.act.op(lambda: nc.scalar.copy(out=e[:, 0:m], in_=pp[:, 0:m]), reads=[pp], writes=[e])
                toks.append(M.sp.dma(outs[name][ts * P:(ts + 1) * P, c0:c0 + m], e[:, 0:m], reads=[e]))
                n_t += 1
            nv += 1
    return M.finish(toks)


class _view:
    def __init__(self, parent, o, n):
        self.parent, self.o, self.n = parent, o, n
        self.name = parent.name
    @property
    def writer(self): return self.parent.writer
    @writer.setter
    def writer(self, v): self.parent.writer = v
    @property
    def readers(self): return self.parent.readers
    @readers.setter
    def readers(self, v): self.parent.readers = v
    def __getitem__(self, idx):
        rows, cols = idx
        assert isinstance(cols, slice)
        return self.parent.ap[rows, self.o + cols.start:self.o + cols.stop]


WINS = (2, 4, 8, 16)
HALO = 15
NEG = -1.0e30


def build_B(D, F, NSLOT, NHD, NHS, TOPK, do_ffn=True):
    M = MK(); nc = M.nc
    T = NSLOT * P
    S = 8 * T
    KD = D // P
    assert D == POOLW + HD * (NHD + NHS) and NHD % 4 == 0 and NHS % 4 == 0
    SW = P + HALO
    hT = M.din("hT", [D, T])
    up_h = M.din("up_h", [POOLW, NSLOT * SW], BF16)
    invcnt = M.din("invcnt", [P, 4 * T])
    pool_w = M.din("pool_w", [4 * 256, 256])
    pscale = M.din("pscale", [P, 8])
    ident_d = M.din("ident", [P, P], BF16)
    trineg_d = M.din("trineg", [P, P], BF16)
    qbT = M.din("qbT", [NHD * HD, T], BF16)
    qiT = M.din("qiT", [1024, T], BF16)
    wi = M.din("wi", [T, 16])
    qcT = M.din("qcT", [NHS * HD, T], BF16)
    kbT_all = M.din("kbT_all", [NHD * HD, S], BF16)
    vb_all = M.din("vb_all", [S, NHD * HD], BF16)
    kiT_all = M.din("kiT_all", [64, S], BF16)
    kcT_all = M.din("kcT_all", [NHS * HD, S], BF16)
    vc_all = M.din("vc_all", [S, NHS * HD], BF16)
    biasraw = M.din("biasraw", [P, 9 * NHD * HD])
    biasfar = M.din("biasfar", [P, NHD * HD])
    sbmask_d = M.din("sbmask", [P, 8 * 512], BF16)
    idxneg_d = M.din("idxneg", [P, 8 * P])
    w_out = M.din("w_out", [D, D])
    gains2 = M.din("gains2", [P, KD])
    wg = M.din("wg", [D, F]); wu = M.din("wu", [D, F]); wd = M.din("wd", [F, D])
    outT = M.dout("outT", [D, T])
    mT_d = M.dscratch("mT_d", [D, T], BF16)
    h2T = M.dscratch("h2T", [D, T], F32)
    mtoks = []

    C = {}; load_consts(M, C)
    C["one"] = M.sb("onec", [P, 1], F32)
    M.dve.op(lambda: nc.vector.memset(C["one"][:], 1.0), writes=[C["one"]])
    C["onesneg"] = M.sb("onesneg", [P, P], BF16)
    M.dve.op(lambda: nc.vector.memset(C["onesneg"][:], -1.0), writes=[C["onesneg"]])
    ident = M.sb("ident_sb", [P, P], BF16)
    M.sp.dma(ident[:], ident_d[:, :], writes=[ident])
    trineg = M.sb("trineg_sb", [P, P], BF16)
    M.sp.dma(trineg[:], trineg_d[:, :], writes=[trineg])

    with M.phase():
        ub = [M.sb(f"p_ub{i}", [P, NSLOT * SW], BF16) for i in range(2)]
        sa = [M.sb(f"p_sa{i}", [P, NSLOT * SW], F32) for i in range(3)]
        icn = M.sb("p_icn", [P, 4 * T], F32)
        M.sp.dma(icn[:], invcnt[:, :], writes=[icn])
        psc = M.sb("p_psc", [P, 8], F32)
        M.sp.dma(psc[:], pscale[:, :], writes=[psc])
        dT = [M.sb(f"p_dT{k}", [P, T], BF16) for k in range(8)]
        tmp = [M.sb(f"p_tmp{i}", [P, T], F32) for i in range(2)]
        pw = [M.sb(f"p_pw{i}", [P, 2, 256], BF16) for i in range(2)]
        pps = [M.ps(f"p_ps{i}", [P, 512], F32) for i in range(2)]
        pev = [M.sb(f"p_ev{i}", [P, 512], BF16) for i in range(2)]
        v3 = lambda b: b.ap[:, :].rearrange("p (s w) -> p s w", w=SW)
        for k in range(8):
            g = k // 2; w = WINS[g]
            u = ub[k % 2]
            M.sp.dma(u[:], up_h[k * P:(k + 1) * P, :], writes=[u])
            uf = sa[0]
            M.act.op(lambda: nc.scalar.copy(out=uf[:], in_=u[:]), reads=[u], writes=[uf])
            cur, other = uf, 1
            sh = 1
            while sh < w:
                nxt = sa[other]
                c3, n3 = v3(cur), v3(nxt)
                M.dve.op(lambda: nc.vector.tensor_copy(out=n3[:, :, 0:sh], in_=c3[:, :, 0:sh]), reads=[cur], writes=[nxt])
                M.dve.op(lambda: nc.vector.tensor_tensor(out=n3[:, :, sh:SW], in0=c3[:, :, sh:SW], in1=c3[:, :, 0:SW - sh], op=ALU.add),
                         reads=[cur], writes=[nxt])
                cur = nxt
                other = 2 if other == 1 else 1
                sh *= 2
            c3 = v3(cur); u3 = v3(uf)
            t3 = tmp[k % 2]
            t3v = t3.ap[:, :].rearrange("p (s q) -> p s q", q=P)
            icv = icn.ap[:, g * T:(g + 1) * T].rearrange("p (s q) -> p s q", q=P)
            M.dve.op(lambda: nc.vector.tensor_tensor(out=t3v, in0=c3[:, :, HALO:SW], in1=icv, op=ALU.mult),
                     reads=[cur, icn], writes=[t3])
            dv = dT[k].ap[:, :].rearrange("p (s q) -> p s q", q=P)
            M.dve.op(lambda: nc.vector.tensor_tensor(out=dv, in0=t3v, in1=u3[:, :, HALO:SW], op=ALU.subtract),
                     reads=[t3, uf], writes=[dT[k]])
        n_e = 0
        for g in range(4):
            pwb = pw[g % 2]
            M.pool.dma(pwb[:], pool_w[g * 256:(g + 1) * 256, :].rearrange("(k p) f -> p k f", p=P), writes=[pwb])
            for co in range(2):
                for t0 in range(0, T, 512):
                    tn = min(512, T - t0)
                    pp = pps[n_e % 2]
                    for ci in range(2):
                        M.pe.op(lambda: nc.tensor.matmul(pp[:, 0:tn], pwb[:, ci, co * P:(co + 1) * P], dT[2 * g + ci][:, t0:t0 + tn],
                                                         start=(ci == 0), stop=(ci == 1)),
                                reads=[pwb, dT[2 * g + ci]], writes=[pp])
                    e = pev[n_e % 2]
                    ko = 2 * g + co
                    M.dve.op(lambda: nc.vector.tensor_scalar(out=e[:, 0:tn], in0=pp[:, 0:tn], scalar1=psc[:, ko:ko + 1], scalar2=None,
                                                              op0=ALU.mult),
                             reads=[pp, psc], writes=[e])
                    mtoks.append(M.sp.dma(mT_d[ko * P:(ko + 1) * P, t0:t0 + tn], e[:, 0:tn], reads=[e]))
                    n_e += 1

    with M.phase():
        NMAX = S
        ki2 = M.sb("a_ki2", [P, S], BF16)
        M.sp.dma(ki2[0:64, :], kiT_all[:, :], writes=[ki2])
        M.sp.dma(ki2[64:128, :], kiT_all[:, :], writes=[ki2])
        EB = M.sb("a_EB", [P, 9 * NHD * HD], BF16)
        EBf = M.sb("a_EBf", [P, NHD * HD], BF16)
        braw = M.sb("a_braw", [P, NHD * HD], F32)
        for r in range(9):
            M.sp.dma(braw[:], biasraw[:, r * NHD * HD:(r + 1) * NHD * HD], writes=[braw])
            M.act.op(lambda: nc.scalar.activation(out=EB[:, r * NHD * HD:(r + 1) * NHD * HD], in_=braw[:], func=AF.Exp),
                     reads=[braw], writes=[EB])
        M.sp.dma(braw[:], biasfar[:, :], writes=[braw])
        M.act.op(lambda: nc.scalar.activation(out=EBf[:], in_=braw[:], func=AF.Exp), reads=[braw], writes=[EBf])
        sbm = M.sb("a_sbm", [P, 8 * 512], BF16)
        M.sp.dma(sbm[:], sbmask_d[:, :], writes=[sbm])
        ineg = M.sb("a_ineg", [P, 8 * P], F32)
        M.sp.dma(ineg[:], idxneg_d[:, :], writes=[ineg])
        idx = M.sb("a_idx", [P, NMAX], F32)
        sel = M.sb("a_sel", [P, NMAX], BF16)
        selT = M.sb("a_selT", [P, NMAX], BF16)
        mx8 = M.sb("a_mx8", [P, 8], F32)
        qb_s = M.sb("a_qb", [P, NHD, P], BF16)
        qc_s = M.sb("a_qc", [P, NHS, P], BF16)
        qi_s = M.sb("a_qi", [P, 8, P], BF16)
        wi_s = M.sb("a_wi", [P, 16], F32)
        rl = [M.sb(f"a_rl{i}", [P, 512], F32) for i in range(2)]
        kt = [M.sb(f"a_kt{i}", [P, 4, 4 * P], BF16) for i in range(2)]
        vt = [M.sb(f"a_vt{i}", [P, 4, 4 * HD], BF16) for i in range(2)]
        pS = [M.sb(f"a_p{i}", [P, 512], F32) for i in range(2)]
        EM = [M.sb(f"a_EM{i}", [P, 512], BF16) for i in range(2)]
        pm = [M.sb(f"a_pm{i}", [P, 512], BF16) for i in range(2)]
        rinv = M.sb("a_rinv", [P, 512], F32)
        oev = [M.sb(f"a_oev{i}", [P, 512], BF16) for i in range(2)]
        eS = [M.sb(f"a_e{i}", [P, 512], F32) for i in range(2)]
        spS = [M.sb(f"a_sp{i}", [P, 512], F32) for i in range(2)]
        spb = [M.sb(f"a_spb{i}", [P, 512], BF16) for i in range(2)]
        t1 = [M.sb(f"a_t1{i}", [P, 512], F32) for i in range(2)]
        t2 = [M.sb(f"a_t2{i}", [P, 512], F32) for i in range(2)]
        aS = [M.sb(f"a_a{i}", [P, 512], BF16) for i in range(2)]
        carry = M.sb("a_carry", [P, 512], F32)
        PB = [M.ps(f"a_pb{i}", [P, 512], F32) for i in range(7)]
        PT = M.ps("a_pt", [P, 4 * P], BF16)
        sc_dsa = float(HD) ** -0.5
        for s in range(NSLOT):
            J = 8 * (s + 1)
            n = J * P
            ts = slice(s * P, (s + 1) * P)
            M.sp.dma(qb_s[:], qbT[:, ts].rearrange("(h p) t -> p h t", p=P), writes=[qb_s])
            M.sp.dma(qc_s[:], qcT[:, ts].rearrange("(h p) t -> p h t", p=P), writes=[qc_s])
            M.sp.dma(qi_s[:], qiT[:, ts].rearrange("(h p) t -> p h t", p=P), writes=[qi_s])
            M.sp.dma(wi_s[:], wi[ts, :], writes=[wi_s])
            for kc in range(n // 512):
                acc = idx.ap[:, kc * 512:(kc + 1) * 512]
                for h in range(16):
                    sc = PB[h % 2]
                    pb0 = (h % 2) * 64
                    M.pe.op(lambda: nc.tensor.matmul(sc[:], qi_s[pb0:pb0 + 64, h // 2, :], ki2[pb0:pb0 + 64, kc * 512:(kc + 1) * 512],
                                                     start=True, stop=True),
                            reads=[qi_s, ki2], writes=[sc])
                    r_ = rl[h % 2]
                    M.act.op(lambda: nc.scalar.activation(out=r_[:], in_=sc[:], func=AF.Relu), reads=[sc], writes=[r_])
                    if h == 0:
                        M.dve.op(lambda: nc.vector.tensor_scalar(out=acc, in0=r_[:], scalar1=wi_s[:, 0:1], scalar2=None, op0=ALU.mult),
                                 reads=[r_, wi_s], writes=[idx])
                    else:
                        M.dve.op(lambda: nc.vector.scalar_tensor_tensor(out=acc, in0=r_[:], scalar=wi_s[:, h:h + 1], in1=acc,
                                                                          op0=ALU.mult, op1=ALU.add),
                                 reads=[r_, wi_s, idx], writes=[idx])
            M.dve.op(lambda: nc.vector.tensor_tensor(out=idx.ap[:, n - 8 * P:n], in0=idx.ap[:, n - 8 * P:n], in1=ineg[:], op=ALU.add),
                     reads=[idx, ineg], writes=[idx])
            for _ in range(TOPK // 8):
                M.dve.op(lambda: nc.vector.max(out=mx8[:], in_=idx.ap[:, 0:n]), reads=[idx], writes=[mx8])
                M.dve.op(lambda: nc.vector.match_replace(out=idx.ap[:, 0:n], in_to_replace=mx8[:], in_values=idx.ap[:, 0:n], imm_value=NEG),
                         reads=[idx, mx8], writes=[idx])
            M.dve.op(lambda: nc.vector.tensor_single_scalar(out=sel.ap[:, 0:n], in_=idx.ap[:, 0:n], scalar=-1.0e29, op=ALU.is_le),
                     reads=[idx], writes=[sel])
            for j0 in range(0, J, 4):
                for jj in range(4):
                    j = j0 + jj
                    M.pe.op(lambda: nc.tensor.transpose(PT[:, jj * P:(jj + 1) * P], sel.ap[:, j * P:(j + 1) * P], ident[:]),
                            reads=[sel, ident], writes=[PT])
                M.act.op(lambda: nc.scalar.copy(out=selT.ap[:, j0 * P:(j0 + 4) * P], in_=PT[:]), reads=[PT], writes=[selT])
            for g in range(NHD // 4):
                o_ps, rs_ps = PB[6], PB[5]
                for j in range(J - 1, -1, -1):
                    it = j % 2
                    r = j - 8 * s
                    cj, jj = j // 4, j % 4
                    k_t, v_t = kt[cj % 2], vt[cj % 2]
                    if jj == 3:
                        M.sp.dma(k_t[:], kbT_all[g * 512:(g + 1) * 512, cj * 512:(cj + 1) * 512].rearrange("(h p) t -> p h t", p=P), writes=[k_t])
                        M.sp.dma(v_t[:], vb_all[cj * 512:(cj + 1) * 512, g * 512:(g + 1) * 512].rearrange("(j p) c -> p j c", p=P), writes=[v_t])
                    l_ps = PB[it]
                    for hh in range(4):
                        M.pe.op(lambda: nc.tensor.matmul(l_ps[:, hh * P:(hh + 1) * P], k_t[:, hh, jj * P:(jj + 1) * P], qb_s[:, 4 * g + hh, :], start=True, stop=True),
                                reads=[k_t, qb_s], writes=[l_ps])
                    p_ = pS[it]
                    M.act.op(lambda: nc.scalar.activation(out=p_[:], in_=l_ps[:], func=AF.Exp, scale=sc_dsa), reads=[l_ps], writes=[p_])
                    if r <= -2:
                        ebv = EBf.ap[:, g * 512:(g + 1) * 512]; ebb = EBf
                    else:
                        base = (r + 1) * NHD * HD + g * 512
                        ebv = EB.ap[:, base:base + 512]; ebb = EB
                    em = EM[it]
                    M.dve.op(lambda: nc.vector.tensor_tensor(out=em.ap[:, :].rearrange("p (h q) -> p h q", q=P),
                                                             in0=ebv.rearrange("p (h q) -> p h q", q=P),
                                                             in1=selT.ap[:, j * P:(j + 1) * P].unsqueeze(1).to_broadcast([P, 4, P]),
                                                             op=ALU.mult),
                             reads=[ebb, selT], writes=[em])
                    pm_ = pm[it]
                    M.dve.op(lambda: nc.vector.tensor_tensor(out=pm_[:], in0=p_[:], in1=em[:], op=ALU.mult), reads=[p_, em], writes=[pm_])
                    for hh in range(4):
                        M.pe.op(lambda: nc.tensor.matmul(o_ps[:, hh * P:(hh + 1) * P], v_t[:, jj, hh * HD:(hh + 1) * HD], pm_[:, hh * P:(hh + 1) * P],
                                                         start=(j == J - 1 and hh == 0), stop=(j == 0 and hh == 3)),
                                reads=[v_t, pm_], writes=[o_ps])
                    M.pe.op(lambda: nc.tensor.matmul(rs_ps[:], C["ones_bf"][:], pm_[:], start=(j == J - 1), stop=(j == 0)),
                            reads=[pm_, C["ones_bf"]], writes=[rs_ps])
                M.dve.op(lambda: nc.vector.reciprocal(out=rinv[:], in_=rs_ps[:]), reads=[rs_ps], writes=[rinv])
                oe = oev[g % 2]
                M.dve.op(lambda: nc.vector.tensor_tensor(out=oe[:], in0=o_ps[:], in1=rinv[:], op=ALU.mult), reads=[o_ps, rinv], writes=[oe])
                for hh in range(4):
                    row = POOLW + (4 * g + hh) * HD
                    mtoks.append(M.sp.dma(mT_d[row:row + HD, ts], oe[:, hh * P:(hh + 1) * P], reads=[oe]))
            for g in range(NHS // 4):
                o_ps = PB[6]
                M.dve.op(lambda: nc.vector.memset(carry[:], 0.0), writes=[carry])
                for j in range(J - 1, -1, -1):
                    it = j % 2
                    r = j - 8 * s
                    cj, jj = j // 4, j % 4
                    k_t, v_t = kt[cj % 2], vt[cj % 2]
                    if jj == 3:
                        M.sp.dma(k_t[:], kcT_all[g * 512:(g + 1) * 512, cj * 512:(cj + 1) * 512].rearrange("(h p) t -> p h t", p=P), writes=[k_t])
                        M.sp.dma(v_t[:], vc_all[cj * 512:(cj + 1) * 512, g * 512:(g + 1) * 512].rearrange("(j p) c -> p j c", p=P), writes=[v_t])
                    z_ps, tri_ps, cs_ps = PB[it], PB[2 + it], PB[4 + (it if False else 0)]
                    for hh in range(4):
                        M.pe.op(lambda: nc.tensor.matmul(z_ps[:, hh * P:(hh + 1) * P], k_t[:, hh, jj * P:(jj + 1) * P], qc_s[:, 4 * g + hh, :], start=True, stop=True),
                                reads=[k_t, qc_s], writes=[z_ps])
                    e_, sp_, spb_ = eS[it], spS[it], spb[it]
                    M.act.op(lambda: nc.scalar.activation(out=e_[:], in_=z_ps[:], func=AF.Exp, scale=sc_dsa), reads=[z_ps], writes=[e_])
                    M.act.op(lambda: nc.scalar.activation(out=sp_[:], in_=e_[:], func=AF.Ln, bias=C["one"][:], scale=1.0),
                             reads=[e_, C["one"]], writes=[sp_])
                    if r >= 0:
                        M.pool.op(lambda: nc.gpsimd.tensor_tensor(out=spb_[:], in0=sp_[:], in1=sbm.ap[:, r * 512:(r + 1) * 512], op=ALU.mult),
                                  reads=[sp_, sbm], writes=[spb_])
                    else:
                        M.pool.op(lambda: nc.gpsimd.tensor_copy(out=spb_[:], in_=sp_[:]), reads=[sp_], writes=[spb_])
                    M.pe.op(lambda: nc.tensor.matmul(tri_ps[:], trineg[:], spb_[:], start=True, stop=True), reads=[trineg, spb_], writes=[tri_ps])
                    M.pe.op(lambda: nc.tensor.matmul(cs_ps[:], C["onesneg"][:], spb_[:], start=True, stop=True),
                            reads=[C["onesneg"], spb_], writes=[cs_ps])
                    t1_, t2_, a_ = t1[it], t2[it], aS[it]
                    M.dve.op(lambda: nc.vector.scalar_tensor_tensor(out=t1_[:], in0=z_ps[:], scalar=sc_dsa, in1=sp_[:], op0=ALU.mult, op1=ALU.subtract),
                             reads=[z_ps, sp_], writes=[t1_])
                    M.dve.op(lambda: nc.vector.tensor_tensor(out=t2_[:], in0=tri_ps[:], in1=t1_[:], op=ALU.add), reads=[tri_ps, t1_], writes=[t2_])
                    M.pool.op(lambda: nc.gpsimd.tensor_tensor(out=t2_[:], in0=t2_[:], in1=carry[:], op=ALU.add), reads=[t2_, carry], writes=[t2_])
                    M.act.op(lambda: nc.scalar.activation(out=a_[:], in_=t2_[:], func=AF.Exp), reads=[t2_], writes=[a_])
                    if r >= 0:
                        M.pool.op(lambda: nc.gpsimd.tensor_tensor(out=a_[:], in0=a_[:], in1=sbm.ap[:, r * 512:(r + 1) * 512], op=ALU.mult),
                                  reads=[a_, sbm], writes=[a_])
                    M.dve.op(lambda: nc.vector.tensor_tensor(out=carry[:], in0=cs_ps[:], in1=carry[:], op=ALU.add), reads=[cs_ps, carry], writes=[carry])
                    for hh in range(4):
                        M.pe.op(lambda: nc.tensor.matmul(o_ps[:, hh * P:(hh + 1) * P], v_t[:, jj, hh * HD:(hh + 1) * HD], a_[:, hh * P:(hh + 1) * P],
                                                         start=(j == J - 1 and hh == 0), stop=(j == 0 and hh == 3)),
                                reads=[v_t, a_], writes=[o_ps])
                oe = oev[g % 2]
                M.act.op(lambda: nc.scalar.copy(out=oe[:], in_=o_ps[:]), reads=[o_ps], writes=[oe])
                for hh in range(4):
                    row = POOLW + NHD * HD + (4 * g + hh) * HD
                    mtoks.append(M.sp.dma(mT_d[row:row + HD, ts], oe[:, hh * P:(hh + 1) * P], reads=[oe]))

    M.sp._wait(mtoks)
    toks = []
    with M.phase():
        Tw = max(T, TT)
        W = make_ffn_work2(M, D, F, Tw)
        gs = M.sb("gains2_sb", [P, KD], F32)
        M.sp.dma(gs[:], gains2[:, :], writes=[gs])
        xn = W["xn"]
        M.sp.dma(xn[:, :, 0:T], mT_d[:, :].rearrange("(k p) t -> p k t", p=P), writes=[xn])
        htoks = []
        dst = h2T if do_ffn else outT
        its = [(i, t0) for i in range(KD) for t0 in range(0, T, TT)]
        def load_h(n):
            i, t0 = its[n]
            tn = min(TT, T - t0)
            xk = W["xk"][n % 3]
            M.sp.dma(xk[:, 0:tn], hT[i * P:(i + 1) * P, t0:t0 + tn], writes=[xk])
            return xk
        xks = {0: load_h(0)}
        for n, (i, t0) in enumerate(its):
            if t0 == 0:
                bw = W["wg"][i % 2]
                M.pool.dma(bw[:], w_out[:, i * P:(i + 1) * P].rearrange("(k p) f -> p k f", p=P), writes=[bw])
            if n + 1 < len(its):
                xks[n + 1] = load_h(n + 1)
            tn = min(TT, T - t0)
            xk = xks.pop(n)
            py = W["pb"][n % 4]
            for k in range(KD):
                M.pe.op(lambda: nc.tensor.matmul(py[:, 0:tn], bw[:, k, :], xn[:, k, t0:t0 + tn], start=(k == 0), stop=(k == KD - 1)),
                        reads=[bw, xn], writes=[py])
            yo = W["yo"][n % 2]
            M.dve.op(lambda: nc.vector.tensor_tensor(out=yo[:, 0:tn], in0=py[:, 0:tn], in1=xk[:, 0:tn], op=ALU.add),
                     reads=[py, xk], writes=[yo])
            htoks.append(M.sp.dma(dst[i * P:(i + 1) * P, t0:t0 + tn], yo[:, 0:tn], reads=[yo]))
        M.sp._wait(htoks)
        toks += htoks
        if do_ffn:
            toks += ffn_full(M, C, W, h2T, gs, wg, wu, wd, outT, D, F, T)
    return M.finish(toks)

BF = ml_dtypes.bfloat16
P = 128
NEG = -1.0e30


def rel_bucket_np(n):
    n = np.maximum(n, 0)
    nf = np.maximum(n, 1).astype(np.float32)
    large = 16 + (np.log(nf / np.float32(16)) / np.float32(math.log(128 / 16)) * np.float32(16)).astype(np.int32)
    large = np.minimum(large, 31)
    return np.where(n < 16, n, large)


def core_tokens(c, NSLOT):
    return np.concatenate([np.arange((8 * s + c) * P, (8 * s + c + 1) * P) for s in range(NSLOT)])


def core_consts(c, NSLOT, rel_bias, NHD):
    k = np.arange(P)[:, None]; q = np.arange(P)[None, :]
    braw = np.zeros((P, 9, NHD, P), np.float32)
    for ri, r in enumerate(range(-1, 8)):
        dist = 128 * (c - r) + q - k
        b = rel_bias[rel_bucket_np(dist)]
        b = np.where((dist >= 0)[:, :, None], b, np.float32(-30000.0))
        braw[:, ri] = np.transpose(b, (0, 2, 1))[:, :NHD]
    bfar = np.broadcast_to(rel_bias[31][None, :NHD, None], (P, NHD, P)).astype(np.float32)
    sbm = np.zeros((P, 8, 4, P), np.float32)
    ineg = np.zeros((P, 8, P), np.float32)
    for r in range(8):
        dist = 128 * (c - r) + q - k
        sbm[:, r] = (dist > 0)[:, None, :]
        ineg[:, r] = np.where(dist.T >= 0, 0.0, NEG)
    T = NSLOT * P
    gt = core_tokens(c, NSLOT)
    inv = np.stack([1.0 / np.minimum(gt + 1, w) for w in (2, 4, 8, 16)]).astype(np.float32)
    inv = np.broadcast_to(inv.reshape(1, 4 * T), (P, 4 * T))
    return {"biasraw": np.ascontiguousarray(braw.reshape(P, -1)), "biasfar": np.ascontiguousarray(bfar.reshape(P, -1)),
            "sbmask": np.ascontiguousarray(sbm.reshape(P, -1)).astype(BF), "idxneg": np.ascontiguousarray(ineg.reshape(P, -1)),
            "invcnt": np.ascontiguousarray(inv)}


def shared_consts():
    j = np.arange(P)[:, None]; s = np.arange(P)[None, :]
    return {"ident": np.eye(P, dtype=np.float32).astype(BF), "trineg": np.where(j > s, -1.0, 0.0).astype(np.float32).astype(BF)}


def up_halo(upT_glob, c, NSLOT):
    out = np.zeros((upT_glob.shape[0], NSLOT, P + 15), upT_glob.dtype)
    for s in range(NSLOT):
        g0 = (8 * s + c) * P
        lo = max(0, g0 - 15)
        out[:, s, 15 - (g0 - lo):] = upT_glob[:, lo:g0 + P]
    return np.ascontiguousarray(out.reshape(upT_glob.shape[0], -1))


def pk(g, D):
    return np.ascontiguousarray(np.asarray(g, np.float32).reshape(D // P, P).T)


D_MODEL, SEQ, DEPTH, D_FF = 4096, 8192, 4, 6144
NCORE, NSLOT_FULL, NHD_FULL, NHS_FULL, TOPK_FULL = 8, 8, 12, 12, 256


def kernel(x, positions, rel_bias, ffn1_norm, ffn1_gate, ffn1_up, ffn1_down, mix_norm, w_in, pool_w, pool_scale,
           q_norm, k_norm, w_out, ffn2_norm, ffn2_gate, ffn2_up, ffn2_down):
    f32 = np.float32
    D, S, T = D_MODEL, SEQ, NSLOT_FULL * P
    x = np.asarray(x, f32)
    rel_bias = np.asarray(rel_bias, f32)
    ncA = build_A(D, D_FF, T)
    ncB = build_B(D, D_FF, NSLOT_FULL, NHD_FULL, NHS_FULL, TOPK_FULL, do_ffn=True)
    toks = [core_tokens(c, NSLOT_FULL) for c in range(NCORE)]
    consts = [core_consts(c, NSLOT_FULL, rel_bias, NHD_FULL) for c in range(NCORE)]
    sh = shared_consts()
    xT = [np.ascontiguousarray(x[0, toks[c], :].T) for c in range(NCORE)]
    cores = list(range(NCORE))
    for i in range(DEPTH):
        gains = np.concatenate([pk(ffn1_norm[i], D), pk(mix_norm[i], D)], 1)
        qkg = np.ascontiguousarray(np.stack([np.asarray(q_norm[i], f32), np.asarray(k_norm[i], f32)], 1))
        wA = {"gains": gains, "qkg": qkg, "wg": np.asarray(ffn1_gate[i], f32), "wu": np.asarray(ffn1_up[i], f32),
              "wd": np.asarray(ffn1_down[i], f32), "w_in": np.asarray(w_in[i], f32)}
        _t = time.time()
        ra = run_bass_kernel_spmd(ncA, [dict(wA, xT=xT[c]) for c in cores], core_ids=cores).results
        print(f'[kernel] layer {i} A {time.time() - _t:.1f}s', flush=True)
        def gather_T(name):
            out = np.empty((ra[0][name].shape[0], S), ra[0][name].dtype)
            for c in cores:
                out[:, toks[c]] = ra[c][name]
            return out
        def gather_R(name):
            out = np.empty((S, ra[0][name].shape[1]), ra[0][name].dtype)
            for c in cores:
                out[toks[c], :] = ra[c][name]
            return out
        upT_g = gather_T("upT")
        kv = {"kbT_all": gather_T("kbT"), "vb_all": gather_R("vb"), "kiT_all": gather_T("kiT"),
              "kcT_all": gather_T("kcT"), "vc_all": gather_R("vc")}
        wB = {"pool_w": np.ascontiguousarray(np.asarray(pool_w[i], f32).reshape(1024, 256)), "pscale": pk(pool_scale[i], 1024),
              "w_out": np.asarray(w_out[i], f32), "gains2": pk(ffn2_norm[i], D), "wg": np.asarray(ffn2_gate[i], f32),
              "wu": np.asarray(ffn2_up[i], f32), "wd": np.asarray(ffn2_down[i], f32)}
        wB.update(sh); wB.update(kv)
        in_b = []
        for c in cores:
            d = dict(wB); d.update(consts[c])
            d.update({"hT": ra[c]["hT"], "up_h": up_halo(upT_g, c, NSLOT_FULL), "qbT": ra[c]["qbT"], "qiT": ra[c]["qiT"],
                      "wi": ra[c]["wi"], "qcT": ra[c]["qcT"]})
            in_b.append(d)
        del ra
        _t = time.time()
        rb = run_bass_kernel_spmd(ncB, in_b, core_ids=cores).results
        print(f'[kernel] layer {i} B {time.time() - _t:.1f}s', flush=True)
        xT = [rb[c]["outT"] for c in cores]
        del rb, in_b
    out = np.empty((1, S, D), f32)
    for c in cores:
        out[0, toks[c], :] = xT[c].T
    return out
```

```python
import math
import time
from contextlib import ExitStack
import numpy as np
import ml_dtypes
import concourse.bass as bass
import concourse.mybir as mybir
from concourse.bass_utils import run_bass_kernel_spmd


F32 = mybir.dt.float32
BF16 = mybir.dt.bfloat16
AF = mybir.ActivationFunctionType
ALU = mybir.AluOpType
AX = mybir.AxisListType

SEM_ROLL = 30000


class Sem:
    def __init__(self, M, name):
        self.M, self.name = M, name
        self.h = M.sem_es.enter_context(M.nc.semaphore(name))
        self.count = 0


class Buf:
    def __init__(self, name, ap=None):
        self.name, self.ap = name, ap
        self.writer = None
        self.readers = []

    def __getitem__(self, idx):
        return self.ap[idx]


class Eng:
    def __init__(self, M, name, e, is_pe=False):
        self.M, self.name, self.e, self.is_pe = M, name, e, is_pe
        self.nsem = 0
        self.sem = None
        self.waited = {}
        self.ndma = 0
        self.dsems = []
        self._roll()

    def _roll(self):
        self.sem = Sem(self.M, f"s_{self.name}_{self.nsem}")
        self.nsem += 1

    def _wait(self, deps):
        best = {}
        for tok in deps:
            if tok is None:
                continue
            s, v = tok
            if best.get(s, 0) < v:
                best[s] = v
        for s, v in best.items():
            if self.is_pe and s is self.sem:
                continue
            if self.waited.get(s, 0) >= v:
                continue
            self.e.wait_ge(s.h, v)
            self.waited[s] = v

    def _deps(self, reads, writes):
        deps = []
        for b in reads:
            deps.append(b.writer)
            if getattr(b, "psum", False):
                deps.extend(t for t in b.readers if t[0] is not self.sem)
        for b in writes:
            deps.append(b.writer)
            deps.extend(b.readers)
        return deps

    def op(self, fn, reads=(), writes=()):
        self._wait(self._deps(reads, writes))
        if self.sem.count >= SEM_ROLL:
            self._roll()
        inst = fn()
        self.sem.count += 1
        inst.then_inc(self.sem.h, 1)
        tok = (self.sem, self.sem.count)
        for b in reads:
            b.readers.append(tok)
            if len(b.readers) > 64:
                b.readers = b.readers[-64:] if False else _compact(b.readers)
        for b in writes:
            b.writer = tok
            b.readers = []
        return tok

    def dma(self, out, in_, reads=(), writes=(), nslots=8, **kw):
        if not self.dsems:
            self.dsems = [Sem(self.M, f"d_{self.name}_{i}") for i in range(nslots)]
        s = self.dsems[self.ndma % len(self.dsems)]
        self.ndma += 1
        deps = self._deps(reads, writes)
        if s.count:
            deps.append((s, s.count))
        self._wait(deps)
        inst = self.e.dma_start(out=out, in_=in_, **kw)
        s.count += 16
        inst.then_inc(s.h, 16)
        tok = (s, s.count)
        for b in reads:
            b.readers.append(tok)
        for b in writes:
            b.writer = tok
            b.readers = []
        return tok

    def wait_tok(self, tok):
        self._wait([tok])


def _compact(readers):
    best = {}
    for s, v in readers:
        if best.get(s, 0) < v:
            best[s] = v
    return list(best.items())


class MK:
    def __init__(self):
        self.nc = bass.Bass("TRN2", target_bir_lowering=False)
        self.es = ExitStack()
        self.sem_es = ExitStack()
        nc = self.nc
        self.pe = Eng(self, "pe", nc.tensor, is_pe=True)
        self.act = Eng(self, "act", nc.scalar)
        self.dve = Eng(self, "dve", nc.vector)
        self.pool = Eng(self, "pool", nc.gpsimd)
        self.sp = Eng(self, "sp", nc.sync)
        self.nbank = 0

    def din(self, name, shape, dt=F32):
        return self.nc.dram_tensor(name, list(shape), dt, kind="ExternalInput").ap()

    def dout(self, name, shape, dt=F32):
        return self.nc.dram_tensor(name, list(shape), dt, kind="ExternalOutput").ap()

    def dscratch(self, name, shape, dt=F32):
        return self.nc.dram_tensor(name, list(shape), dt).ap()

    def sb(self, name, shape, dt=F32):
        t = self.es.enter_context(self.nc.sbuf_tensor(name, list(shape), dt))
        return Buf(name, t)

    def ps(self, name, shape, dt=F32):
        t = self.es.enter_context(self.nc.psum_tensor(name, list(shape), dt))
        b = Buf(name, t); b.psum = True
        return b

    def engines(self):
        return [self.pe, self.act, self.dve, self.pool, self.sp]

    def barrier(self):
        toks = []
        for X in self.engines():
            toks.append((X.sem, X.sem.count))
            toks.extend((d, d.count) for d in X.dsems if d.count)
        toks = [t for t in toks if t[1] > 0]
        for X in self.engines():
            X._wait(toks)

    def phase(self):
        M = self
        class _Ph:
            def __enter__(s):
                s.old = M.es; M.es = ExitStack(); return s
            def __exit__(s, *a):
                M.barrier(); M.es.close(); M.es = s.old; return False
        return _Ph()

    def finish(self, toks):
        self.sp._wait(toks)
        self.es.close()
        self.sem_es.close()
        return self.nc


P = 128
TT = 512


def load_consts(M, C):
    C["ones_bf"] = M.sb("ones_bf", [P, P], BF16)
    M.dve.op(lambda: M.nc.vector.memset(C["ones_bf"][:], 1.0), writes=[C["ones_bf"]])
    C["eps"] = M.sb("eps", [P, 1], F32)
    M.dve.op(lambda: M.nc.vector.memset(C["eps"][:], 1e-6), writes=[C["eps"]])


def rms_rstd(M, C, W, src_chunk_loader, nchunks, D, tag):
    nc = M.nc
    ss = W["ps_ss"]
    for k in range(nchunks):
        xk = src_chunk_loader(k)
        sq = W["sq"][k % 2]
        M.act.op(lambda: nc.scalar.activation(out=sq[:], in_=xk[:], func=AF.Square), reads=[xk], writes=[sq])
        M.pe.op(lambda: nc.tensor.matmul(ss[:], C["ones_bf"][:], sq[:], start=(k == 0), stop=(k == nchunks - 1)),
                reads=[sq, C["ones_bf"]], writes=[ss])
    rstd = W["rstd"]
    M.act.op(lambda: nc.scalar.activation(out=rstd[:], in_=ss[:], func=AF.Sqrt, bias=C["eps"][:], scale=1.0 / D),
             reads=[ss, C["eps"]], writes=[rstd])
    M.dve.op(lambda: nc.vector.reciprocal(out=rstd[:], in_=rstd[:]), reads=[rstd], writes=[rstd])
    return rstd


def make_ffn_work(M, D, F):
    KD, KF = D // P, F // P
    W = {}
    W["xk"] = [M.sb(f"f_xk{i}", [P, TT], F32) for i in range(3)]
    W["sq"] = [M.sb(f"f_sq{i}", [P, TT], BF16) for i in range(2)]
    W["rstd"] = M.sb("f_rstd", [P, TT], F32)
    W["xn"] = M.sb("f_xn", [P, KD, TT], BF16)
    W["h"] = M.sb("f_h", [P, KF * TT], BF16)
    W["wg"] = [M.sb(f"f_wg{i}", [P, KD, P], BF16) for i in range(2)]
    W["wu"] = [M.sb(f"f_wu{i}", [P, KD, P], BF16) for i in range(2)]
    W["wd"] = [M.sb(f"f_wd{i}", [P, KF, P], BF16) for i in range(2)]
    W["sg"] = [M.sb(f"f_sg{i}", [P, TT], F32) for i in range(2)]
    W["yo"] = [M.sb(f"f_yo{i}", [P, TT], F32) for i in range(2)]
    W["ps_ss"] = M.ps("ps_ss", [P, TT], F32)
    W["ps_g"] = [M.ps(f"ps_g{i}", [P, TT], F32) for i in range(2)]
    W["ps_u"] = [M.ps(f"ps_u{i}", [P, TT], F32) for i in range(2)]
    W["ps_y"] = [M.ps(f"ps_y{i}", [P, TT], F32) for i in range(2)]
    return W


def norm_tile(M, C, W, xT, gain_sb, D, t0, xn, c0=0):
    nc = M.nc
    KD = D // P
    def loader(k):
        b = W["xk"][k % 3]
        M.sp.dma(b[:], xT[k * P:(k + 1) * P, t0:t0 + TT], writes=[b])
        return b
    rstd = rms_rstd(M, C, W, loader, KD, D, "n")
    for k in range(KD):
        b = loader(k)
        M.dve.op(lambda: nc.vector.scalar_tensor_tensor(out=xn[:, k, c0:c0 + TT], in0=b[:], scalar=gain_sb[:, k:k + 1], in1=rstd[:],
                                                          op0=ALU.mult, op1=ALU.mult),
                 reads=[b, gain_sb, rstd], writes=[xn])


def ffn_tile(M, C, W, xT, gain_sb, wg, wu, wd, outT, D, F, t0):
    nc = M.nc
    KD, KF = D // P, F // P
    xn, h = W["xn"], W["h"]
    norm_tile(M, C, W, xT, gain_sb, D, t0, xn)
    for j in range(KF):
        bg, bu = W["wg"][j % 2], W["wu"][j % 2]
        M.pool.dma(bg[:], wg[:, j * P:(j + 1) * P].rearrange("(k p) f -> p k f", p=P), writes=[bg])
        M.pool.dma(bu[:], wu[:, j * P:(j + 1) * P].rearrange("(k p) f -> p k f", p=P), writes=[bu])
        pg, pu = W["ps_g"][j % 2], W["ps_u"][j % 2]
        for k in range(KD):
            M.pe.op(lambda: nc.tensor.matmul(pg[:], bg[:, k, :], xn[:, k, :], start=(k == 0), stop=(k == KD - 1)),
                    reads=[bg, xn], writes=[pg])
        for k in range(KD):
            M.pe.op(lambda: nc.tensor.matmul(pu[:], bu[:, k, :], xn[:, k, :], start=(k == 0), stop=(k == KD - 1)),
                    reads=[bu, xn], writes=[pu])
        sg = W["sg"][j % 2]
        M.act.op(lambda: nc.scalar.activation(out=sg[:], in_=pg[:], func=AF.Silu), reads=[pg], writes=[sg])
        M.dve.op(lambda: nc.vector.tensor_tensor(out=h[:, j * TT:(j + 1) * TT], in0=sg[:], in1=pu[:], op=ALU.mult),
                 reads=[sg, pu], writes=[h])
    toks = []
    for i in range(KD):
        bd = W["wd"][i % 2]
        M.pool.dma(bd[:], wd[:, i * P:(i + 1) * P].rearrange("(j p) c -> p j c", p=P), writes=[bd])
        xk = W["xk"][i % 3]
        M.sp.dma(xk[:], xT[i * P:(i + 1) * P, t0:t0 + TT], writes=[xk])
        py = W["ps_y"][i % 2]
        for j in range(KF):
            M.pe.op(lambda: nc.tensor.matmul(py[:], bd[:, j, :], h[:, j * TT:(j + 1) * TT], start=(j == 0), stop=(j == KF - 1)),
                    reads=[bd, h], writes=[py])
        yo = W["yo"][i % 2]
        M.dve.op(lambda: nc.vector.scalar_tensor_tensor(out=yo[:], in0=py[:], scalar=0.5, in1=xk[:], op0=ALU.mult, op1=ALU.add),
                 reads=[py, xk], writes=[yo])
        toks.append(M.sp.dma(outT[i * P:(i + 1) * P, t0:t0 + TT], yo[:], reads=[yo]))
    return toks


def make_ffn_work2(M, D, F, T):
    KD, KF = D // P, F // P
    KH = KF // 2
    W = {}
    W["xk"] = [M.sb(f"f_xk{i}", [P, TT], F32) for i in range(3)]
    W["sq"] = [M.sb(f"f_sq{i}", [P, TT], BF16) for i in range(2)]
    W["rstd"] = M.sb("f_rstd", [P, TT], F32)
    W["xn"] = M.sb("f_xn", [P, KD, T], BF16)
    W["h"] = M.sb("f_h", [P, KH * T], BF16)
    W["wg"] = [M.sb(f"f_wg{i}", [P, KD, P], BF16) for i in range(2)]
    W["wu"] = [M.sb(f"f_wu{i}", [P, KD, P], BF16) for i in range(2)]
    W["wd"] = [M.sb(f"f_wd{i}", [P, KH, P], BF16) for i in range(2)]
    W["sg"] = [M.sb(f"f_sg{i}", [P, TT], F32) for i in range(2)]
    W["yo"] = [M.sb(f"f_yo{i}", [P, TT], F32) for i in range(2)]
    W["pb"] = [M.ps(f"f_pb{i}", [P, TT], F32) for i in range(8)]
    W["ps_ss"] = W["pb"][7]
    return W


def ffn_full(M, C, W, xT, gain_sb, wg, wu, wd, outT, D, F, T):
    nc = M.nc
    KD, KF = D // P, F // P
    KH = KF // 2
    NT = T // TT
    xn, h, PB = W["xn"], W["h"], W["pb"]
    for th in range(NT):
        norm_tile(M, C, W, xT, gain_sb, D, th * TT, xn, c0=th * TT)
    part = {}
    toks = []
    n_ev = 0
    n_dn = 0
    for half in range(2):
        for jj in range(KH):
            j = half * KH + jj
            bg, bu = W["wg"][jj % 2], W["wu"][jj % 2]
            M.pool.dma(bg[:], wg[:, j * P:(j + 1) * P].rearrange("(k p) f -> p k f", p=P), writes=[bg])
            M.pool.dma(bu[:], wu[:, j * P:(j + 1) * P].rearrange("(k p) f -> p k f", p=P), writes=[bu])
            for th in range(NT):
                pg = PB[(jj % 2) * 4 + (th % 2) * 2]
                pu = PB[(jj % 2) * 4 + (th % 2) * 2 + 1]
                tc = slice(th * TT, (th + 1) * TT)
                for k in range(KD):
                    M.pe.op(lambda: nc.tensor.matmul(pg[:], bg[:, k, :], xn[:, k, tc], start=(k == 0), stop=(k == KD - 1)),
                            reads=[bg, xn], writes=[pg])
                for k in range(KD):
                    M.pe.op(lambda: nc.tensor.matmul(pu[:], bu[:, k, :], xn[:, k, tc], start=(k == 0), stop=(k == KD - 1)),
                            reads=[bu, xn], writes=[pu])
                sg = W["sg"][n_ev % 2]; n_ev += 1
                M.act.op(lambda: nc.scalar.activation(out=sg[:], in_=pg[:], func=AF.Silu), reads=[pg], writes=[sg])
                hc = slice(jj * T + th * TT, jj * T + (th + 1) * TT)
                M.dve.op(lambda: nc.vector.tensor_tensor(out=h[:, hc], in0=sg[:], in1=pu[:], op=ALU.mult), reads=[sg, pu], writes=[h])
        its = [(i, th) for i in range(KD) for th in range(NT)]
        def load_x(n):
            i, th = its[n]
            tc = slice(th * TT, (th + 1) * TT)
            xk = W["xk"][(n_dn0 + n) % 3]
            if half == 0:
                M.sp.dma(xk[:], xT[i * P:(i + 1) * P, tc], writes=[xk])
            else:
                M.sp._wait([part[(i, th)]])
                M.sp.dma(xk[:], outT[i * P:(i + 1) * P, tc], writes=[xk])
            return xk
        n_dn0 = n_dn
        xks = {0: load_x(0)}
        for n, (i, th) in enumerate(its):
            if th == 0:
                bd = W["wd"][i % 2]
                M.pool.dma(bd[:], wd[half * KH * P:(half + 1) * KH * P, i * P:(i + 1) * P].rearrange("(j p) c -> p j c", p=P), writes=[bd])
            if n + 1 < len(its):
                xks[n + 1] = load_x(n + 1)
            tc = slice(th * TT, (th + 1) * TT)
            xk = xks.pop(n)
            py = PB[n_dn % 4]
            for jj in range(KH):
                hc = slice(jj * T + th * TT, jj * T + (th + 1) * TT)
                M.pe.op(lambda: nc.tensor.matmul(py[:], bd[:, jj, :], h[:, hc], start=(jj == 0), stop=(jj == KH - 1)),
                        reads=[bd, h], writes=[py])
            yo = W["yo"][n_dn % 2]
            M.dve.op(lambda: nc.vector.scalar_tensor_tensor(out=yo[:], in0=py[:], scalar=0.5, in1=xk[:], op0=ALU.mult, op1=ALU.add),
                     reads=[py, xk], writes=[yo])
            tok = M.sp.dma(outT[i * P:(i + 1) * P, tc], yo[:], reads=[yo])
            if half == 0:
                part[(i, th)] = tok
            else:
                toks.append(tok)
            n_dn += 1
    return toks


POOLW, DSAW, SBW, IDXH, IDXD = 1024, 1536, 1536, 16, 64
HD = 128


def col_offsets(D):
    sizes = [("up", POOLW), ("qb", DSAW), ("kb", DSAW), ("vb", DSAW), ("qi", IDXH * IDXD), ("ki", IDXD), ("wi", IDXH),
             ("qc", SBW), ("kc", SBW), ("vc", SBW)]
    off, o = {}, 0
    for n, s in sizes:
        off[n] = (o, s); o += s
    return off, o


def build_A(D, F, T, sizes=None, feat=("up", "qb", "kb", "qi", "ki", "qc", "kc"), tokm=("vb", "vc", "wi")):
    M = MK(); nc = M.nc
    KD = D // P
    off, INW = col_offsets(D)
    if sizes:
        off, INW = sizes
    xT = M.din("xT", [D, T])
    gains = M.din("gains", [P, 2 * KD])
    qkg = M.din("qkg", [P, 2])
    wg = M.din("wg", [D, F]); wu = M.din("wu", [D, F]); wd = M.din("wd", [F, D])
    w_in = M.din("w_in", [D, INW])
    hT = M.dout("hT", [D, T])
    outs = {}
    for n in ("up", "qb", "kb", "qi", "ki", "qc", "kc"):
        outs[n] = M.dout(n + "T", [off[n][1], T], BF16)
    for n in ("vb", "vc"):
        outs[n] = M.dout(n, [T, off[n][1]], BF16)
    outs["wi"] = M.dout("wi", [T, off["wi"][1]], F32)

    C = {}; load_consts(M, C)
    W = make_ffn_work2(M, D, F, T)
    NT = T // TT
    gs = M.sb("gains_sb", [P, 2 * KD], F32)
    M.sp.dma(gs[:], gains[:, :], writes=[gs])
    qk = M.sb("qkg_sb", [P, 2], F32)
    M.sp.dma(qk[:], qkg[:, :], writes=[qk])
    ev = [M.sb(f"a_ev{i}", [P, TT], BF16) for i in range(2)]
    qf = [M.sb(f"a_qf{i}", [P, TT], F32) for i in range(2)]
    evw = [M.sb(f"a_evw{i}", [P, 16], F32) for i in range(2)]
    PB = W["pb"]
    toks = []
    ftoks = ffn_full(M, C, W, xT, _view(gs, 0, KD), wg, wu, wd, hT, D, F, T)
    toks += ftoks
    M.sp._wait(ftoks)
    g_mix = _view(gs, KD, KD)
    xn = W["xn"]
    for th in range(NT):
        norm_tile(M, C, W, hT, g_mix, D, th * TT, xn, c0=th * TT)
    n_ev = 0
    n_w = 0
    for name in feat:
        o, sz = off[name]
        for c0 in range(0, sz, P):
            m = min(P, sz - c0)
            bw = W["wg"][n_w % 2]; n_w += 1
            M.pool.dma(bw[:, :, 0:m], w_in[:, o + c0:o + c0 + m].rearrange("(k p) f -> p k f", p=P), writes=[bw])
            for th in range(NT):
                tc = slice(th * TT, (th + 1) * TT)
                pp = PB[n_ev % 4]
                for k in range(KD):
                    M.pe.op(lambda: nc.tensor.matmul(pp[0:m, :], bw[:, k, 0:m], xn[:, k, tc], start=(k == 0), stop=(k == KD - 1)),
                            reads=[bw, xn], writes=[pp])
                e = ev[n_ev % 2]
                if name in ("qb", "kb"):
                    gi = 0 if name == "qb" else 1
                    f = qf[n_ev % 2]; sq = W["sq"][n_ev % 2]; ss = PB[4 + n_ev % 2]; rstd = W["rstd"]
                    M.dve.op(lambda: nc.vector.tensor_copy(out=f[:], in_=pp[:]), reads=[pp], writes=[f])
                    M.act.op(lambda: nc.scalar.activation(out=sq[:], in_=f[:], func=AF.Square), reads=[f], writes=[sq])
                    M.pe.op(lambda: nc.tensor.matmul(ss[:], C["ones_bf"][:], sq[:], start=True, stop=True),
                            reads=[sq, C["ones_bf"]], writes=[ss])
                    M.act.op(lambda: nc.scalar.activation(out=rstd[:], in_=ss[:], func=AF.Sqrt, bias=C["eps"][:], scale=1.0 / HD),
                             reads=[ss, C["eps"]], writes=[rstd])
                    M.dve.op(lambda: nc.vector.reciprocal(out=rstd[:], in_=rstd[:]), reads=[rstd], writes=[rstd])
                    M.dve.op(lambda: nc.vector.scalar_tensor_tensor(out=e[:], in0=f[:], scalar=qk[:, gi:gi + 1], in1=rstd[:],
                                                                      op0=ALU.mult, op1=ALU.mult),
                             reads=[f, qk, rstd], writes=[e])
                else:
                    M.act.op(lambda: nc.scalar.copy(out=e[0:m, :], in_=pp[0:m, :]), reads=[pp], writes=[e])
                toks.append(M.sp.dma(outs[name][c0:c0 + m, tc], e[0:m, :], reads=[e]))
                n_ev += 1
    h = W["h"]
    CW = 256
    nv = 0
    n_t = 0
    for name in tokm:
        o, sz = off[name]
        for c0 in range(0, sz, CW):
            m = min(CW, sz - c0)
            base = (nv % 2) * KD * CW
            wv = h.ap[:, base:base + KD * CW].rearrange("p (k c) -> p k c", k=KD)
            M.pool.dma(wv[:, :, 0:m], w_in[:, o + c0:o + c0 + m].rearrange("(k p) f -> p k f", p=P), writes=[h])
            for ts in range(T // P):
                pp = PB[n_t % 4]
                for k in range(KD):
                    M.pe.op(lambda: nc.tensor.matmul(pp[:, 0:m], xn[:, k, ts * P:(ts + 1) * P], wv[:, k, 0:m],
                                                     start=(k == 0), stop=(k == KD - 1)),
                            reads=[h, xn], writes=[pp])
                e = evw[n_t % 2] if name == "wi" else ev[n_t % 2]
                M.act.op(lambda: nc.scalar.copy(out=e[:, 0:m], in_=pp[:, 0:m]), reads=[pp], writes=[e])
                toks.append(M.sp.dma(outs[name][ts * P:(ts + 1) * P, c0:c0 + m], e[:, 0:m], reads=[e]))
                n_t += 1
            nv += 1
    return M.finish(toks)


class _view:
    def __init__(self, parent, o, n):
        self.parent, self.o, self.n = parent, o, n
        self.name = parent.name
    @property
    def writer(self): return self.parent.writer
    @writer.setter
    def writer(self, v): self.parent.writer = v
    @property
    def readers(self): return self.parent.readers
    @readers.setter
    def readers(self, v): self.parent.readers = v
    def __getitem__(self, idx):
        rows, cols = idx
        assert isinstance(cols, slice)
        return self.parent.ap[rows, self.o + cols.start:self.o + cols.stop]


WINS = (2, 4, 8, 16)
HALO = 15
NEG = -1.0e30


def build_B(D, F, NSLOT, NHD, NHS, TOPK, do_ffn=True):
    M = MK(); nc = M.nc
    T = NSLOT * P
    S = 8 * T
    KD = D // P
    assert D == POOLW + HD * (NHD + NHS) and NHD % 4 == 0 and NHS % 4 == 0
    SW = P + HALO
    hT = M.din("hT", [D, T])
    up_h = M.din("up_h", [POOLW, NSLOT * SW], BF16)
    invcnt = M.din("invcnt", [P, 4 * T])
    pool_w = M.din("pool_w", [4 * 256, 256])
    pscale = M.din("pscale", [P, 8])
    ident_d = M.din("ident", [P, P], BF16)
    trineg_d = M.din("trineg", [P, P], BF16)
    qbT = M.din("qbT", [NHD * HD, T], BF16)
    qiT = M.din("qiT", [1024, T], BF16)
    wi = M.din("wi", [T, 16])
    qcT = M.din("qcT", [NHS * HD, T], BF16)
    kbT_all = M.din("kbT_all", [NHD * HD, S], BF16)
    vb_all = M.din("vb_all", [S, NHD * HD], BF16)
    kiT_all = M.din("kiT_all", [64, S], BF16)
    kcT_all = M.din("kcT_all", [NHS * HD, S], BF16)
    vc_all = M.din("vc_all", [S, NHS * HD], BF16)
    biasraw = M.din("biasraw", [P, 9 * NHD * HD])
    biasfar = M.din("biasfar", [P, NHD * HD])
    sbmask_d = M.din("sbmask", [P, 8 * 512], BF16)
    idxneg_d = M.din("idxneg", [P, 8 * P])
    w_out = M.din("w_out", [D, D])
    gains2 = M.din("gains2", [P, KD])
    wg = M.din("wg", [D, F]); wu = M.din("wu", [D, F]); wd = M.din("wd", [F, D])
    outT = M.dout("outT", [D, T])
    mT_d = M.dscratch("mT_d", [D, T], BF16)
    h2T = M.dscratch("h2T", [D, T], F32)
    mtoks = []

    C = {}; load_consts(M, C)
    C["one"] = M.sb("onec", [P, 1], F32)
    M.dve.op(lambda: nc.vector.memset(C["one"][:], 1.0), writes=[C["one"]])
    C["onesneg"] = M.sb("onesneg", [P, P], BF16)
    M.dve.op(lambda: nc.vector.memset(C["onesneg"][:], -1.0), writes=[C["onesneg"]])
    ident = M.sb("ident_sb", [P, P], BF16)
    M.sp.dma(ident[:], ident_d[:, :], writes=[ident])
    trineg = M.sb("trineg_sb", [P, P], BF16)
    M.sp.dma(trineg[:], trineg_d[:, :], writes=[trineg])

    with M.phase():
        ub = [M.sb(f"p_ub{i}", [P, NSLOT * SW], BF16) for i in range(2)]
        sa = [M.sb(f"p_sa{i}", [P, NSLOT * SW], F32) for i in range(3)]
        icn = M.sb("p_icn", [P, 4 * T], F32)
        M.sp.dma(icn[:], invcnt[:, :], writes=[icn])
        psc = M.sb("p_psc", [P, 8], F32)
        M.sp.dma(psc[:], pscale[:, :], writes=[psc])
        dT = [M.sb(f"p_dT{k}", [P, T], BF16) for k in range(8)]
        tmp = [M.sb(f"p_tmp{i}", [P, T], F32) for i in range(2)]
        pw = [M.sb(f"p_pw{i}", [P, 2, 256], BF16) for i in range(2)]
        pps = [M.ps(f"p_ps{i}", [P, 512], F32) for i in range(2)]
        pev = [M.sb(f"p_ev{i}", [P, 512], BF16) for i in range(2)]
        v3 = lambda b: b.ap[:, :].rearrange("p (s w) -> p s w", w=SW)
        for k in range(8):
            g = k // 2; w = WINS[g]
            u = ub[k % 2]
            M.sp.dma(u[:], up_h[k * P:(k + 1) * P, :], writes=[u])
            uf = sa[0]
            M.act.op(lambda: nc.scalar.copy(out=uf[:], in_=u[:]), reads=[u], writes=[uf])
            cur, other = uf, 1
            sh = 1
            while sh < w:
                nxt = sa[other]
                c3, n3 = v3(cur), v3(nxt)
                M.dve.op(lambda: nc.vector.tensor_copy(out=n3[:, :, 0:sh], in_=c3[:, :, 0:sh]), reads=[cur], writes=[nxt])
                M.dve.op(lambda: nc.vector.tensor_tensor(out=n3[:, :, sh:SW], in0=c3[:, :, sh:SW], in1=c3[:, :, 0:SW - sh], op=ALU.add),
                         reads=[cur], writes=[nxt])
                cur = nxt
                other = 2 if other == 1 else 1
                sh *= 2
            c3 = v3(cur); u3 = v3(uf)
            t3 = tmp[k % 2]
            t3v = t3.ap[:, :].rearrange("p (s q) -> p s q", q=P)
            icv = icn.ap[:, g * T:(g + 1) * T].rearrange("p (s q) -> p s q", q=P)
            M.dve.op(lambda: nc.vector.tensor_tensor(out=t3v, in0=c3[:, :, HALO:SW], in1=icv, op=ALU.mult),
                     reads=[cur, icn], writes=[t3])
            dv = dT[k].ap[:, :].rearrange("p (s q) -> p s q", q=P)
            M.dve.op(lambda: nc.vector.tensor_tensor(out=dv, in0=t3v, in1=u3[:, :, HALO:SW], op=ALU.subtract),
                     reads=[t3, uf], writes=[dT[k]])
        n_e = 0
        for g in range(4):
            pwb = pw[g % 2]
            M.pool.dma(pwb[:], pool_w[g * 256:(g + 1) * 256, :].rearrange("(k p) f -> p k f", p=P), writes=[pwb])
            for co in range(2):
                for t0 in range(0, T, 512):
                    tn = min(512, T - t0)
                    pp = pps[n_e % 2]
                    for ci in range(2):
                        M.pe.op(lambda: nc.tensor.matmul(pp[:, 0:tn], pwb[:, ci, co * P:(co + 1) * P], dT[2 * g + ci][:, t0:t0 + tn],
                                                         start=(ci == 0), stop=(ci == 1)),
                                reads=[pwb, dT[2 * g + ci]], writes=[pp])
                    e = pev[n_e % 2]
                    ko = 2 * g + co
                    M.dve.op(lambda: nc.vector.tensor_scalar(out=e[:, 0:tn], in0=pp[:, 0:tn], scalar1=psc[:, ko:ko + 1], scalar2=None,
                                                              op0=ALU.mult),
                             reads=[pp, psc], writes=[e])
                    mtoks.append(M.sp.dma(mT_d[ko * P:(ko + 1) * P, t0:t0 + tn], e[:, 0:tn], reads=[e]))
                    n_e += 1

    with M.phase():
        NMAX = S
        ki2 = M.sb("a_ki2", [P, S], BF16)
        M.sp.dma(ki2[0:64, :], kiT_all[:, :], writes=[ki2])
        M.sp.dma(ki2[64:128, :], kiT_all[:, :], writes=[ki2])
        EB = M.sb("a_EB", [P, 9 * NHD * HD], BF16)
        EBf = M.sb("a_EBf", [P, NHD * HD], BF16)
        braw = M.sb("a_braw", [P, NHD * HD], F32)
        for r in range(9):
            M.sp.dma(braw[:], biasraw[:, r * NHD * HD:(r + 1) * NHD * HD], writes=[braw])
            M.act.op(lambda: nc.scalar.activation(out=EB[:, r * NHD * HD:(r + 1) * NHD * HD], in_=braw[:], func=AF.Exp),
                     reads=[braw], writes=[EB])
        M.sp.dma(braw[:], biasfar[:, :], writes=[braw])
        M.act.op(lambda: nc.scalar.activation(out=EBf[:], in_=braw[:], func=AF.Exp), reads=[braw], writes=[EBf])
        sbm = M.sb("a_sbm", [P, 8 * 512], BF16)
        M.sp.dma(sbm[:], sbmask_d[:, :], writes=[sbm])
        ineg = M.sb("a_ineg", [P, 8 * P], F32)
        M.sp.dma(ineg[:], idxneg_d[:, :], writes=[ineg])
        idx = M.sb("a_idx", [P, NMAX], F32)
        sel = M.sb("a_sel", [P, NMAX], BF16)
        selT = M.sb("a_selT", [P, NMAX], BF16)
        mx8 = M.sb("a_mx8", [P, 8], F32)
        qb_s = M.sb("a_qb", [P, NHD, P], BF16)
        qc_s = M.sb("a_qc", [P, NHS, P], BF16)
        qi_s = M.sb("a_qi", [P, 8, P], BF16)
        wi_s = M.sb("a_wi", [P, 16], F32)
        rl = [M.sb(f"a_rl{i}", [P, 512], F32) for i in range(2)]
        kt = [M.sb(f"a_kt{i}", [P, 4, 4 * P], BF16) for i in range(2)]
        vt = [M.sb(f"a_vt{i}", [P, 4, 4 * HD], BF16) for i in range(2)]
        pS = [M.sb(f"a_p{i}", [P, 512], F32) for i in range(2)]
        EM = [M.sb(f"a_EM{i}", [P, 512], BF16) for i in range(2)]
        pm = [M.sb(f"a_pm{i}", [P, 512], BF16) for i in range(2)]
        rinv = M.sb("a_rinv", [P, 512], F32)
        oev = [M.sb(f"a_oev{i}", [P, 512], BF16) for i in range(2)]
        eS = [M.sb(f"a_e{i}", [P, 512], F32) for i in range(2)]
        spS = [M.sb(f"a_sp{i}", [P, 512], F32) for i in range(2)]
        spb = [M.sb(f"a_spb{i}", [P, 512], BF16) for i in range(2)]
        t1 = [M.sb(f"a_t1{i}", [P, 512], F32) for i in range(2)]
        t2 = [M.sb(f"a_t2{i}", [P, 512], F32) for i in range(2)]
        aS = [M.sb(f"a_a{i}", [P, 512], BF16) for i in range(2)]
        carry = M.sb("a_carry", [P, 512], F32)
        PB = [M.ps(f"a_pb{i}", [P, 512], F32) for i in range(7)]
        PT = M.ps("a_pt", [P, 4 * P], BF16)
        sc_dsa = float(HD) ** -0.5
        for s in range(NSLOT):
            J = 8 * (s + 1)
            n = J * P
            ts = slice(s * P, (s + 1) * P)
            M.sp.dma(qb_s[:], qbT[:, ts].rearrange("(h p) t -> p h t", p=P), writes=[qb_s])
            M.sp.dma(qc_s[:], qcT[:, ts].rearrange("(h p) t -> p h t", p=P), writes=[qc_s])
            M.sp.dma(qi_s[:], qiT[:, ts].rearrange("(h p) t -> p h t", p=P), writes=[qi_s])
            M.sp.dma(wi_s[:], wi[ts, :], writes=[wi_s])
            for kc in range(n // 512):
                acc = idx.ap[:, kc * 512:(kc + 1) * 512]
                for h in range(16):
                    sc = PB[h % 2]
                    pb0 = (h % 2) * 64
                    M.pe.op(lambda: nc.tensor.matmul(sc[:], qi_s[pb0:pb0 + 64, h // 2, :], ki2[pb0:pb0 + 64, kc * 512:(kc + 1) * 512],
                                                     start=True, stop=True),
                            reads=[qi_s, ki2], writes=[sc])
                    r_ = rl[h % 2]
                    M.act.op(lambda: nc.scalar.activation(out=r_[:], in_=sc[:], func=AF.Relu), reads=[sc], writes=[r_])
                    if h == 0:
                        M.dve.op(lambda: nc.vector.tensor_scalar(out=acc, in0=r_[:], scalar1=wi_s[:, 0:1], scalar2=None, op0=ALU.mult),
                                 reads=[r_, wi_s], writes=[idx])
                    else:
                        M.dve.op(lambda: nc.vector.scalar_tensor_tensor(out=acc, in0=r_[:], scalar=wi_s[:, h:h + 1], in1=acc,
                                                                          op0=ALU.mult, op1=ALU.add),
                                 reads=[r_, wi_s, idx], writes=[idx])
            M.dve.op(lambda: nc.vector.tensor_tensor(out=idx.ap[:, n - 8 * P:n], in0=idx.ap[:, n - 8 * P:n], in1=ineg[:], op=ALU.add),
                     reads=[idx, ineg], writes=[idx])
            for _ in range(TOPK // 8):
                M.dve.op(lambda: nc.vector.max(out=mx8[:], in_=idx.ap[:, 0:n]), reads=[idx], writes=[mx8])
                M.dve.op(lambda: nc.vector.match_replace(out=idx.ap[:, 0:n], in_to_replace=mx8[:], in_values=idx.ap[:, 0:n], imm_value=NEG),
                         reads=[idx, mx8], writes=[idx])
            M.dve.op(lambda: nc.vector.tensor_single_scalar(out=sel.ap[:, 0:n], in_=idx.ap[:, 0:n], scalar=-1.0e29, op=ALU.is_le),
                     reads=[idx], writes=[sel])
            for j0 in range(0, J, 4):
                for jj in range(4):
                    j = j0 + jj
                    M.pe.op(lambda: nc.tensor.transpose(PT[:, jj * P:(jj + 1) * P], sel.ap[:, j * P:(j + 1) * P], ident[:]),
                            reads=[sel, ident], writes=[PT])
                M.act.op(lambda: nc.scalar.copy(out=selT.ap[:, j0 * P:(j0 + 4) * P], in_=PT[:]), reads=[PT], writes=[selT])
            for g in range(NHD // 4):
                o_ps, rs_ps = PB[6], PB[5]
                def D1(j, g=g):
                    it = j % 2
                    r = j - 8 * s
                    cj, jj = j // 4, j % 4
                    k_t, v_t = kt[cj % 2], vt[cj % 2]
                    if jj == 3:
                        M.sp.dma(k_t[:], kbT_all[g * 512:(g + 1) * 512, cj * 512:(cj + 1) * 512].rearrange("(h p) t -> p h t", p=P), writes=[k_t])
                        M.sp.dma(v_t[:], vb_all[cj * 512:(cj + 1) * 512, g * 512:(g + 1) * 512].rearrange("(j p) c -> p j c", p=P), writes=[v_t])
                    l_ps = PB[it]
                    for hh in range(4):
                        M.pe.op(lambda: nc.tensor.matmul(l_ps[:, hh * P:(hh + 1) * P], k_t[:, hh, jj * P:(jj + 1) * P], qb_s[:, 4 * g + hh, :], start=True, stop=True),
                                reads=[k_t, qb_s], writes=[l_ps])
                    p_ = pS[it]
                    M.act.op(lambda: nc.scalar.activation(out=p_[:], in_=l_ps[:], func=AF.Exp, scale=sc_dsa), reads=[l_ps], writes=[p_])
                    if r <= -2:
                        ebv = EBf.ap[:, g * 512:(g + 1) * 512]; ebb = EBf
                    else:
                        base = (r + 1) * NHD * HD + g * 512
                        ebv = EB.ap[:, base:base + 512]; ebb = EB
                    em = EM[it]
                    M.pool.op(lambda: nc.gpsimd.tensor_tensor(out=em.ap[:, :].rearrange("p (h q) -> p h q", q=P),
                                                              in0=ebv.rearrange("p (h q) -> p h q", q=P),
                                                              in1=selT.ap[:, j * P:(j + 1) * P].unsqueeze(1).to_broadcast([P, 4, P]),
                                                              op=ALU.mult),
                              reads=[ebb, selT], writes=[em])
                def D2(j, g=g):
                    it = j % 2
                    cj, jj = j // 4, j % 4
                    v_t = vt[cj % 2]
                    p_, em, pm_ = pS[it], EM[it], pm[it]
                    M.dve.op(lambda: nc.vector.tensor_tensor(out=pm_[:], in0=p_[:], in1=em[:], op=ALU.mult), reads=[p_, em], writes=[pm_])
                    for hh in range(4):
                        M.pe.op(lambda: nc.tensor.matmul(o_ps[:, hh * P:(hh + 1) * P], v_t[:, jj, hh * HD:(hh + 1) * HD], pm_[:, hh * P:(hh + 1) * P],
                                                         start=(j == J - 1 and hh == 0), stop=(j == 0 and hh == 3)),
                                reads=[v_t, pm_], writes=[o_ps])
                    M.pe.op(lambda: nc.tensor.matmul(rs_ps[:], C["ones_bf"][:], pm_[:], start=(j == J - 1), stop=(j == 0)),
                            reads=[pm_, C["ones_bf"]], writes=[rs_ps])
                order = list(range(J - 1, -1, -1))
                for n_, j in enumerate(order):
                    D1(j)
                    if n_ >= 1:
                        D2(order[n_ - 1])
                D2(order[-1])
                M.dve.op(lambda: nc.vector.reciprocal(out=rinv[:], in_=rs_ps[:]), reads=[rs_ps], writes=[rinv])
                oe = oev[g % 2]
                M.dve.op(lambda: nc.vector.tensor_tensor(out=oe[:], in0=o_ps[:], in1=rinv[:], op=ALU.mult), reads=[o_ps, rinv], writes=[oe])
                for hh in range(4):
                    row = POOLW + (4 * g + hh) * HD
                    mtoks.append(M.sp.dma(mT_d[row:row + HD, ts], oe[:, hh * P:(hh + 1) * P], reads=[oe]))
            for g in range(NHS // 4):
                o_ps = PB[6]
                M.dve.op(lambda: nc.vector.memset(carry[:], 0.0), writes=[carry])
                def S1(j, g=g):
                    it = j % 2
                    r = j - 8 * s
                    cj, jj = j // 4, j % 4
                    k_t, v_t = kt[cj % 2], vt[cj % 2]
                    if jj == 3:
                        M.sp.dma(k_t[:], kcT_all[g * 512:(g + 1) * 512, cj * 512:(cj + 1) * 512].rearrange("(h p) t -> p h t", p=P), writes=[k_t])
                        M.sp.dma(v_t[:], vc_all[cj * 512:(cj + 1) * 512, g * 512:(g + 1) * 512].rearrange("(j p) c -> p j c", p=P), writes=[v_t])
                    z_ps, tri_ps, cs_ps = PB[it], PB[2 + it], PB[4 + it]
                    for hh in range(4):
                        M.pe.op(lambda: nc.tensor.matmul(z_ps[:, hh * P:(hh + 1) * P], k_t[:, hh, jj * P:(jj + 1) * P], qc_s[:, 4 * g + hh, :], start=True, stop=True),
                                reads=[k_t, qc_s], writes=[z_ps])
                    e_, sp_, spb_ = eS[it], spS[it], spb[it]
                    M.act.op(lambda: nc.scalar.activation(out=e_[:], in_=z_ps[:], func=AF.Exp, scale=sc_dsa), reads=[z_ps], writes=[e_])
                    M.act.op(lambda: nc.scalar.activation(out=sp_[:], in_=e_[:], func=AF.Ln, bias=C["one"][:], scale=1.0),
                             reads=[e_, C["one"]], writes=[sp_])
                    if r >= 0:
                        M.pool.op(lambda: nc.gpsimd.tensor_tensor(out=spb_[:], in0=sp_[:], in1=sbm.ap[:, r * 512:(r + 1) * 512], op=ALU.mult),
                                  reads=[sp_, sbm], writes=[spb_])
                    else:
                        M.pool.op(lambda: nc.gpsimd.tensor_copy(out=spb_[:], in_=sp_[:]), reads=[sp_], writes=[spb_])
                    M.pe.op(lambda: nc.tensor.matmul(tri_ps[:], trineg[:], spb_[:], start=True, stop=True), reads=[trineg, spb_], writes=[tri_ps])
                    M.pe.op(lambda: nc.tensor.matmul(cs_ps[:], C["onesneg"][:], spb_[:], start=True, stop=True),
                            reads=[C["onesneg"], spb_], writes=[cs_ps])
                    t1_ = t1[it]
                    M.dve.op(lambda: nc.vector.scalar_tensor_tensor(out=t1_[:], in0=z_ps[:], scalar=sc_dsa, in1=sp_[:], op0=ALU.mult, op1=ALU.subtract),
                             reads=[z_ps, sp_], writes=[t1_])
                def S2(j, g=g):
                    it = j % 2
                    r = j - 8 * s
                    cj, jj = j // 4, j % 4
                    v_t = vt[cj % 2]
                    tri_ps, cs_ps = PB[2 + it], PB[4 + it]
                    t1_, t2_, a_ = t1[it], t2[it], aS[it]
                    M.dve.op(lambda: nc.vector.tensor_tensor(out=t2_[:], in0=tri_ps[:], in1=t1_[:], op=ALU.add), reads=[tri_ps, t1_], writes=[t2_])
                    M.dve.op(lambda: nc.vector.tensor_tensor(out=t2_[:], in0=t2_[:], in1=carry[:], op=ALU.add), reads=[t2_, carry], writes=[t2_])
                    M.act.op(lambda: nc.scalar.activation(out=a_[:], in_=t2_[:], func=AF.Exp), reads=[t2_], writes=[a_])
                    if r >= 0:
                        M.pool.op(lambda: nc.gpsimd.tensor_tensor(out=a_[:], in0=a_[:], in1=sbm.ap[:, r * 512:(r + 1) * 512], op=ALU.mult),
                                  reads=[a_, sbm], writes=[a_])
                    M.dve.op(lambda: nc.vector.tensor_tensor(out=carry[:], in0=cs_ps[:], in1=carry[:], op=ALU.add), reads=[cs_ps, carry], writes=[carry])
                    for hh in range(4):
                        M.pe.op(lambda: nc.tensor.matmul(o_ps[:, hh * P:(hh + 1) * P], v_t[:, jj, hh * HD:(hh + 1) * HD], a_[:, hh * P:(hh + 1) * P],
                                                         start=(j == J - 1 and hh == 0), stop=(j == 0 and hh == 3)),
                                reads=[v_t, a_], writes=[o_ps])
                order = list(range(J - 1, -1, -1))
                for n_, j in enumerate(order):
                    S1(j)
                    if n_ >= 1:
                        S2(order[n_ - 1])
                S2(order[-1])
                oe = oev[g % 2]
                M.act.op(lambda: nc.scalar.copy(out=oe[:], in_=o_ps[:]), reads=[o_ps], writes=[oe])
                for hh in range(4):
                    row = POOLW + NHD * HD + (4 * g + hh) * HD
                    mtoks.append(M.sp.dma(mT_d[row:row + HD, ts], oe[:, hh * P:(hh + 1) * P], reads=[oe]))

    M.sp._wait(mtoks)
    toks = []
    with M.phase():
        Tw = max(T, TT)
        W = make_ffn_work2(M, D, F, Tw)
        gs = M.sb("gains2_sb", [P, KD], F32)
        M.sp.dma(gs[:], gains2[:, :], writes=[gs])
        xn = W["xn"]
        M.sp.dma(xn[:, :, 0:T], mT_d[:, :].rearrange("(k p) t -> p k t", p=P), writes=[xn])
        htoks = []
        dst = h2T if do_ffn else outT
        its = [(i, t0) for i in range(KD) for t0 in range(0, T, TT)]
        def load_h(n):
            i, t0 = its[n]
            tn = min(TT, T - t0)
            xk = W["xk"][n % 3]
            M.sp.dma(xk[:, 0:tn], hT[i * P:(i + 1) * P, t0:t0 + tn], writes=[xk])
            return xk
        xks = {0: load_h(0)}
        for n, (i, t0) in enumerate(its):
            if t0 == 0:
                bw = W["wg"][i % 2]
                M.pool.dma(bw[:], w_out[:, i * P:(i + 1) * P].rearrange("(k p) f -> p k f", p=P), writes=[bw])
            if n + 1 < len(its):
                xks[n + 1] = load_h(n + 1)
            tn = min(TT, T - t0)
            xk = xks.pop(n)
            py = W["pb"][n % 4]
            for k in range(KD):
                M.pe.op(lambda: nc.tensor.matmul(py[:, 0:tn], bw[:, k, :], xn[:, k, t0:t0 + tn], start=(k == 0), stop=(k == KD - 1)),
                        reads=[bw, xn], writes=[py])
            yo = W["yo"][n % 2]
            M.dve.op(lambda: nc.vector.tensor_tensor(out=yo[:, 0:tn], in0=py[:, 0:tn], in1=xk[:, 0:tn], op=ALU.add),
                     reads=[py, xk], writes=[yo])
            htoks.append(M.sp.dma(dst[i * P:(i + 1) * P, t0:t0 + tn], yo[:, 0:tn], reads=[yo]))
        M.sp._wait(htoks)
        toks += htoks
        if do_ffn:
            toks += ffn_full(M, C, W, h2T, gs, wg, wu, wd, outT, D, F, T)
    return M.finish(toks)

BF = ml_dtypes.bfloat16
P = 128
NEG = -1.0e30


def rel_bucket_np(n):
    n = np.maximum(n, 0)
    nf = np.maximum(n, 1).astype(np.float32)
    large = 16 + (np.log(nf / np.float32(16)) / np.float32(math.log(128 / 16)) * np.float32(16)).astype(np.int32)
    large = np.minimum(large, 31)
    return np.where(n < 16, n, large)


def core_tokens(c, NSLOT):
    return np.concatenate([np.arange((8 * s + c) * P, (8 * s + c + 1) * P) for s in range(NSLOT)])


def core_consts(c, NSLOT, rel_bias, NHD):
    k = np.arange(P)[:, None]; q = np.arange(P)[None, :]
    braw = np.zeros((P, 9, NHD, P), np.float32)
    for ri, r in enumerate(range(-1, 8)):
        dist = 128 * (c - r) + q - k
        b = rel_bias[rel_bucket_np(dist)]
        b = np.where((dist >= 0)[:, :, None], b, np.float32(-30000.0))
        braw[:, ri] = np.transpose(b, (0, 2, 1))[:, :NHD]
    bfar = np.broadcast_to(rel_bias[31][None, :NHD, None], (P, NHD, P)).astype(np.float32)
    sbm = np.zeros((P, 8, 4, P), np.float32)
    ineg = np.zeros((P, 8, P), np.float32)
    for r in range(8):
        dist = 128 * (c - r) + q - k
        sbm[:, r] = (dist > 0)[:, None, :]
        ineg[:, r] = np.where(dist.T >= 0, 0.0, NEG)
    T = NSLOT * P
    gt = core_tokens(c, NSLOT)
    inv = np.stack([1.0 / np.minimum(gt + 1, w) for w in (2, 4, 8, 16)]).astype(np.float32)
    inv = np.broadcast_to(inv.reshape(1, 4 * T), (P, 4 * T))
    return {"biasraw": np.ascontiguousarray(braw.reshape(P, -1)), "biasfar": np.ascontiguousarray(bfar.reshape(P, -1)),
            "sbmask": np.ascontiguousarray(sbm.reshape(P, -1)).astype(BF), "idxneg": np.ascontiguousarray(ineg.reshape(P, -1)),
            "invcnt": np.ascontiguousarray(inv)}


def shared_consts():
    j = np.arange(P)[:, None]; s = np.arange(P)[None, :]
    return {"ident": np.eye(P, dtype=np.float32).astype(BF), "trineg": np.where(j > s, -1.0, 0.0).astype(np.float32).astype(BF)}


def up_halo(upT_glob, c, NSLOT):
    out = np.zeros((upT_glob.shape[0], NSLOT, P + 15), upT_glob.dtype)
    for s in range(NSLOT):
        g0 = (8 * s + c) * P
        lo = max(0, g0 - 15)
        out[:, s, 15 - (g0 - lo):] = upT_glob[:, lo:g0 + P]
    return np.ascontiguousarray(out.reshape(upT_glob.shape[0], -1))


def pk(g, D):
    return np.ascontiguousarray(np.asarray(g, np.float32).reshape(D // P, P).T)


D_MODEL, SEQ, DEPTH, D_FF = 4096, 8192, 4, 6144
NCORE, NSLOT_FULL, NHD_FULL, NHS_FULL, TOPK_FULL = 8, 8, 12, 12, 256


def kernel(x, positions, rel_bias, ffn1_norm, ffn1_gate, ffn1_up, ffn1_down, mix_norm, w_in, pool_w, pool_scale,
           q_norm, k_norm, w_out, ffn2_norm, ffn2_gate, ffn2_up, ffn2_down):
    f32 = np.float32
    D, S, T = D_MODEL, SEQ, NSLOT_FULL * P
    x = np.asarray(x, f32)
    rel_bias = np.asarray(rel_bias, f32)
    ncA = build_A(D, D_FF, T)
    ncB = build_B(D, D_FF, NSLOT_FULL, NHD_FULL, NHS_FULL, TOPK_FULL, do_ffn=True)
    toks = [core_tokens(c, NSLOT_FULL) for c in range(NCORE)]
    consts = [core_consts(c, NSLOT_FULL, rel_bias, NHD_FULL) for c in range(NCORE)]
    sh = shared_consts()
    xT = [np.ascontiguousarray(x[0, toks[c], :].T) for c in range(NCORE)]
    cores = list(range(NCORE))
    for i in range(DEPTH):
        gains = np.concatenate([pk(ffn1_norm[i], D), pk(mix_norm[i], D)], 1)
        qkg = np.ascontiguousarray(np.stack([np.asarray(q_norm[i], f32), np.asarray(k_norm[i], f32)], 1))
        wA = {"gains": gains, "qkg": qkg, "wg": np.asarray(ffn1_gate[i], f32), "wu": np.asarray(ffn1_up[i], f32),
              "wd": np.asarray(ffn1_down[i], f32), "w_in": np.asarray(w_in[i], f32)}
        _t = time.time()
        ra = run_bass_kernel_spmd(ncA, [dict(wA, xT=xT[c]) for c in cores], core_ids=cores).results
        print(f'[kernel] layer {i} A {time.time() - _t:.1f}s', flush=True)
        def gather_T(name):
            out = np.empty((ra[0][name].shape[0], S), ra[0][name].dtype)
            for c in cores:
                out[:, toks[c]] = ra[c][name]
            return out
        def gather_R(name):
            out = np.empty((S, ra[0][name].shape[1]), ra[0][name].dtype)
            for c in cores:
                out[toks[c], :] = ra[c][name]
            return out
        upT_g = gather_T("upT")
        kv = {"kbT_all": gather_T("kbT"), "vb_all": gather_R("vb"), "kiT_all": gather_T("kiT"),
              "kcT_all": gather_T("kcT"), "vc_all": gather_R("vc")}
        wB = {"pool_w": np.ascontiguousarray(np.asarray(pool_w[i], f32).reshape(1024, 256)), "pscale": pk(pool_scale[i], 1024),
              "w_out": np.asarray(w_out[i], f32), "gains2": pk(ffn2_norm[i], D), "wg": np.asarray(ffn2_gate[i], f32),
              "wu": np.asarray(ffn2_up[i], f32), "wd": np.asarray(ffn2_down[i], f32)}
        wB.update(sh); wB.update(kv)
        in_b = []
        for c in cores:
            d = dict(wB); d.update(consts[c])
            d.update({"hT": ra[c]["hT"], "up_h": up_halo(upT_g, c, NSLOT_FULL), "qbT": ra[c]["qbT"], "qiT": ra[c]["qiT"],
                      "wi": ra[c]["wi"], "qcT": ra[c]["qcT"]})
            in_b.append(d)
        del ra
        _t = time.time()
        rb = run_bass_kernel_spmd(ncB, in_b, core_ids=cores).results
        print(f'[kernel] layer {i} B {time.time() - _t:.1f}s', flush=True)
        xT = [rb[c]["outT"] for c in cores]
        del rb, in_b
    out = np.empty((1, S, D), f32)
    for c in cores:
        out[0, toks[c], :] = xT[c].T
    return out
```

```python
import math
import time
from contextlib import ExitStack
import numpy as np
import ml_dtypes
import concourse.bass as bass
import concourse.mybir as mybir
from concourse.bass_utils import run_bass_kernel_spmd


F32 = mybir.dt.float32
BF16 = mybir.dt.bfloat16
AF = mybir.ActivationFunctionType
ALU = mybir.AluOpType
AX = mybir.AxisListType

SEM_ROLL = 30000


class Sem:
    def __init__(self, M, name):
        self.M, self.name = M, name
        self.h = M.sem_es.enter_context(M.nc.semaphore(name))
        self.count = 0


class Buf:
    def __init__(self, name, ap=None):
        self.name, self.ap = name, ap
        self.writer = None
        self.readers = []

    def __getitem__(self, idx):
        return self.ap[idx]


class Eng:
    def __init__(self, M, name, e, is_pe=False):
        self.M, self.name, self.e, self.is_pe = M, name, e, is_pe
        self.nsem = 0
        self.sem = None
        self.waited = {}
        self.ndma = 0
        self.dsems = []
        self._roll()

    def _roll(self):
        self.sem = Sem(self.M, f"s_{self.name}_{self.nsem}")
        self.nsem += 1

    def _wait(self, deps):
        best = {}
        for tok in deps:
            if tok is None:
                continue
            s, v = tok
            if best.get(s, 0) < v:
                best[s] = v
        for s, v in best.items():
            if self.is_pe and s is self.sem:
                continue
            if self.waited.get(s, 0) >= v:
                continue
            self.e.wait_ge(s.h, v)
            self.waited[s] = v

    def _deps(self, reads, writes):
        deps = []
        for b in reads:
            deps.append(b.writer)
            if getattr(b, "psum", False):
                deps.extend(t for t in b.readers if t[0] is not self.sem)
        for b in writes:
            deps.append(b.writer)
            deps.extend(b.readers)
        return deps

    def op(self, fn, reads=(), writes=()):
        self._wait(self._deps(reads, writes))
        if self.sem.count >= SEM_ROLL:
            self._roll()
        inst = fn()
        self.sem.count += 1
        inst.then_inc(self.sem.h, 1)
        tok = (self.sem, self.sem.count)
        for b in reads:
            b.readers.append(tok)
            if len(b.readers) > 64:
                b.readers = b.readers[-64:] if False else _compact(b.readers)
        for b in writes:
            b.writer = tok
            b.readers = []
        return tok

    def dma(self, out, in_, reads=(), writes=(), nslots=8, **kw):
        if not self.dsems:
            self.dsems = [Sem(self.M, f"d_{self.name}_{i}") for i in range(nslots)]
        s = self.dsems[self.ndma % len(self.dsems)]
        self.ndma += 1
        deps = self._deps(reads, writes)
        if s.count:
            deps.append((s, s.count))
        self._wait(deps)
        inst = self.e.dma_start(out=out, in_=in_, **kw)
        s.count += 16
        inst.then_inc(s.h, 16)
        tok = (s, s.count)
        for b in reads:
            b.readers.append(tok)
        for b in writes:
            b.writer = tok
            b.readers = []
        return tok

    def wait_tok(self, tok):
        self._wait([tok])


def _compact(readers):
    best = {}
    for s, v in readers:
        if best.get(s, 0) < v:
            best[s] = v
    return list(best.items())


class MK:
    def __init__(self):
        self.nc = bass.Bass("TRN2", target_bir_lowering=False)
        self.es = ExitStack()
        self.sem_es = ExitStack()
        nc = self.nc
        self.pe = Eng(self, "pe", nc.tensor, is_pe=True)
        self.act = Eng(self, "act", nc.scalar)
        self.dve = Eng(self, "dve", nc.vector)
        self.pool = Eng(self, "pool", nc.gpsimd)
        self.sp = Eng(self, "sp", nc.sync)
        self.nbank = 0

    def din(self, name, shape, dt=F32):
        return self.nc.dram_tensor(name, list(shape), dt, kind="ExternalInput").ap()

    def dout(self, name, shape, dt=F32):
        return self.nc.dram_tensor(name, list(shape), dt, kind="ExternalOutput").ap()

    def dscratch(self, name, shape, dt=F32):
        return self.nc.dram_tensor(name, list(shape), dt).ap()

    def sb(self, name, shape, dt=F32):
        t = self.es.enter_context(self.nc.sbuf_tensor(name, list(shape), dt))
        return Buf(name, t)

    def ps(self, name, shape, dt=F32):
        t = self.es.enter_context(self.nc.psum_tensor(name, list(shape), dt))
        b = Buf(name, t); b.psum = True
        return b

    def engines(self):
        return [self.pe, self.act, self.dve, self.pool, self.sp]

    def barrier(self):
        toks = []
        for X in self.engines():
            toks.append((X.sem, X.sem.count))
            toks.extend((d, d.count) for d in X.dsems if d.count)
        toks = [t for t in toks if t[1] > 0]
        for X in self.engines():
            X._wait(toks)

    def phase(self):
        M = self
        class _Ph:
            def __enter__(s):
                s.old = M.es; M.es = ExitStack(); return s
            def __exit__(s, *a):
                M.barrier(); M.es.close(); M.es = s.old; return False
        return _Ph()

    def finish(self, toks):
        self.sp._wait(toks)
        self.es.close()
        self.sem_es.close()
        return self.nc


P = 128
TT = 512


def load_consts(M, C):
    C["ones_bf"] = M.sb("ones_bf", [P, P], BF16)
    M.dve.op(lambda: M.nc.vector.memset(C["ones_bf"][:], 1.0), writes=[C["ones_bf"]])
    C["eps"] = M.sb("eps", [P, 1], F32)
    M.dve.op(lambda: M.nc.vector.memset(C["eps"][:], 1e-6), writes=[C["eps"]])


def rms_rstd(M, C, W, src_chunk_loader, nchunks, D, tag):
    nc = M.nc
    ss = W["ps_ss"]
    for k in range(nchunks):
        xk = src_chunk_loader(k)
        sq = W["sq"][k % 2]
        M.act.op(lambda: nc.scalar.activation(out=sq[:], in_=xk[:], func=AF.Square), reads=[xk], writes=[sq])
        M.pe.op(lambda: nc.tensor.matmul(ss[:], C["ones_bf"][:], sq[:], start=(k == 0), stop=(k == nchunks - 1)),
                reads=[sq, C["ones_bf"]], writes=[ss])
    rstd = W["rstd"]
    M.act.op(lambda: nc.scalar.activation(out=rstd[:], in_=ss[:], func=AF.Sqrt, bias=C["eps"][:], scale=1.0 / D),
             reads=[ss, C["eps"]], writes=[rstd])
    M.dve.op(lambda: nc.vector.reciprocal(out=rstd[:], in_=rstd[:]), reads=[rstd], writes=[rstd])
    return rstd


def make_ffn_work(M, D, F):
    KD, KF = D // P, F // P
    W = {}
    W["xk"] = [M.sb(f"f_xk{i}", [P, TT], F32) for i in range(3)]
    W["sq"] = [M.sb(f"f_sq{i}", [P, TT], BF16) for i in range(2)]
    W["rstd"] = M.sb("f_rstd", [P, TT], F32)
    W["xn"] = M.sb("f_xn", [P, KD, TT], BF16)
    W["h"] = M.sb("f_h", [P, KF * TT], BF16)
    W["wg"] = [M.sb(f"f_wg{i}", [P, KD, P], BF16) for i in range(2)]
    W["wu"] = [M.sb(f"f_wu{i}", [P, KD, P], BF16) for i in range(2)]
    W["wd"] = [M.sb(f"f_wd{i}", [P, KF, P], BF16) for i in range(2)]
    W["sg"] = [M.sb(f"f_sg{i}", [P, TT], F32) for i in range(2)]
    W["yo"] = [M.sb(f"f_yo{i}", [P, TT], F32) for i in range(2)]
    W["ps_ss"] = M.ps("ps_ss", [P, TT], F32)
    W["ps_g"] = [M.ps(f"ps_g{i}", [P, TT], F32) for i in range(2)]
    W["ps_u"] = [M.ps(f"ps_u{i}", [P, TT], F32) for i in range(2)]
    W["ps_y"] = [M.ps(f"ps_y{i}", [P, TT], F32) for i in range(2)]
    return W


def norm_tile(M, C, W, xT, gain_sb, D, t0, xn, c0=0):
    nc = M.nc
    KD = D // P
    def loader(k):
        b = W["xk"][k % 3]
        M.sp.dma(b[:], xT[k * P:(k + 1) * P, t0:t0 + TT], writes=[b])
        return b
    rstd = rms_rstd(M, C, W, loader, KD, D, "n")
    for k in range(KD):
        b = loader(k)
        M.dve.op(lambda: nc.vector.scalar_tensor_tensor(out=xn[:, k, c0:c0 + TT], in0=b[:], scalar=gain_sb[:, k:k + 1], in1=rstd[:],
                                                          op0=ALU.mult, op1=ALU.mult),
                 reads=[b, gain_sb, rstd], writes=[xn])


def ffn_tile(M, C, W, xT, gain_sb, wg, wu, wd, outT, D, F, t0):
    nc = M.nc
    KD, KF = D // P, F // P
    xn, h = W["xn"], W["h"]
    norm_tile(M, C, W, xT, gain_sb, D, t0, xn)
    for j in range(KF):
        bg, bu = W["wg"][j % 2], W["wu"][j % 2]
        M.pool.dma(bg[:], wg[:, j * P:(j + 1) * P].rearrange("(k p) f -> p k f", p=P), writes=[bg])
        M.pool.dma(bu[:], wu[:, j * P:(j + 1) * P].rearrange("(k p) f -> p k f", p=P), writes=[bu])
        pg, pu = W["ps_g"][j % 2], W["ps_u"][j % 2]
        for k in range(KD):
            M.pe.op(lambda: nc.tensor.matmul(pg[:], bg[:, k, :], xn[:, k, :], start=(k == 0), stop=(k == KD - 1)),
                    reads=[bg, xn], writes=[pg])
        for k in range(KD):
            M.pe.op(lambda: nc.tensor.matmul(pu[:], bu[:, k, :], xn[:, k, :], start=(k == 0), stop=(k == KD - 1)),
                    reads=[bu, xn], writes=[pu])
        sg = W["sg"][j % 2]
        M.act.op(lambda: nc.scalar.activation(out=sg[:], in_=pg[:], func=AF.Silu), reads=[pg], writes=[sg])
        M.dve.op(lambda: nc.vector.tensor_tensor(out=h[:, j * TT:(j + 1) * TT], in0=sg[:], in1=pu[:], op=ALU.mult),
                 reads=[sg, pu], writes=[h])
    toks = []
    for i in range(KD):
        bd = W["wd"][i % 2]
        M.pool.dma(bd[:], wd[:, i * P:(i + 1) * P].rearrange("(j p) c -> p j c", p=P), writes=[bd])
        xk = W["xk"][i % 3]
        M.sp.dma(xk[:], xT[i * P:(i + 1) * P, t0:t0 + TT], writes=[xk])
        py = W["ps_y"][i % 2]
        for j in range(KF):
            M.pe.op(lambda: nc.tensor.matmul(py[:], bd[:, j, :], h[:, j * TT:(j + 1) * TT], start=(j == 0), stop=(j == KF - 1)),
                    reads=[bd, h], writes=[py])
        yo = W["yo"][i % 2]
        M.dve.op(lambda: nc.vector.scalar_tensor_tensor(out=yo[:], in0=py[:], scalar=0.5, in1=xk[:], op0=ALU.mult, op1=ALU.add),
                 reads=[py, xk], writes=[yo])
        toks.append(M.sp.dma(outT[i * P:(i + 1) * P, t0:t0 + TT], yo[:], reads=[yo]))
    return toks


def make_ffn_work2(M, D, F, T):
    KD, KF = D // P, F // P
    KH = KF // 2
    W = {}
    W["xk"] = [M.sb(f"f_xk{i}", [P, TT], F32) for i in range(3)]
    W["sq"] = [M.sb(f"f_sq{i}", [P, TT], BF16) for i in range(2)]
    W["rstd"] = M.sb("f_rstd", [P, TT], F32)
    W["xn"] = M.sb("f_xn", [P, KD, T], BF16)
    W["h"] = M.sb("f_h", [P, KH * T], BF16)
    W["wg"] = [M.sb(f"f_wg{i}", [P, KD, P], BF16) for i in range(2)]
    W["wu"] = [M.sb(f"f_wu{i}", [P, KD, P], BF16) for i in range(2)]
    W["wd"] = [M.sb(f"f_wd{i}", [P, KH, P], BF16) for i in range(2)]
    W["sg"] = [M.sb(f"f_sg{i}", [P, TT], F32) for i in range(2)]
    W["yo"] = [M.sb(f"f_yo{i}", [P, TT], F32) for i in range(2)]
    W["pb"] = [M.ps(f"f_pb{i}", [P, TT], F32) for i in range(8)]
    W["ps_ss"] = W["pb"][7]
    return W


def ffn_full(M, C, W, xT, gain_sb, wg, wu, wd, outT, D, F, T):
    nc = M.nc
    KD, KF = D // P, F // P
    KH = KF // 2
    NT = T // TT
    xn, h, PB = W["xn"], W["h"], W["pb"]
    for th in range(NT):
        norm_tile(M, C, W, xT, gain_sb, D, th * TT, xn, c0=th * TT)
    part = {}
    toks = []
    n_ev = 0
    n_dn = 0
    for half in range(2):
        for jj in range(KH):
            j = half * KH + jj
            bg, bu = W["wg"][jj % 2], W["wu"][jj % 2]
            M.pool.dma(bg[:], wg[:, j * P:(j + 1) * P].rearrange("(k p) f -> p k f", p=P), writes=[bg])
            M.pool.dma(bu[:], wu[:, j * P:(j + 1) * P].rearrange("(k p) f -> p k f", p=P), writes=[bu])
            for th in range(NT):
                pg = PB[(jj % 2) * 4 + (th % 2) * 2]
                pu = PB[(jj % 2) * 4 + (th % 2) * 2 + 1]
                tc = slice(th * TT, (th + 1) * TT)
                for k in range(KD):
                    M.pe.op(lambda: nc.tensor.matmul(pg[:], bg[:, k, :], xn[:, k, tc], start=(k == 0), stop=(k == KD - 1)),
                            reads=[bg, xn], writes=[pg])
                for k in range(KD):
                    M.pe.op(lambda: nc.tensor.matmul(pu[:], bu[:, k, :], xn[:, k, tc], start=(k == 0), stop=(k == KD - 1)),
                            reads=[bu, xn], writes=[pu])
                sg = W["sg"][n_ev % 2]; n_ev += 1
                M.act.op(lambda: nc.scalar.activation(out=sg[:], in_=pg[:], func=AF.Silu), reads=[pg], writes=[sg])
                hc = slice(jj * T + th * TT, jj * T + (th + 1) * TT)
                M.dve.op(lambda: nc.vector.tensor_tensor(out=h[:, hc], in0=sg[:], in1=pu[:], op=ALU.mult), reads=[sg, pu], writes=[h])
        its = [(i, th) for i in range(KD) for th in range(NT)]
        def load_x(n):
            i, th = its[n]
            tc = slice(th * TT, (th + 1) * TT)
            xk = W["xk"][(n_dn0 + n) % 3]
            if half == 0:
                M.sp.dma(xk[:], xT[i * P:(i + 1) * P, tc], writes=[xk])
            else:
                M.sp._wait([part[(i, th)]])
                M.sp.dma(xk[:], outT[i * P:(i + 1) * P, tc], writes=[xk])
            return xk
        n_dn0 = n_dn
        xks = {0: load_x(0)}
        for n, (i, th) in enumerate(its):
            if th == 0:
                bd = W["wd"][i % 2]
                M.pool.dma(bd[:], wd[half * KH * P:(half + 1) * KH * P, i * P:(i + 1) * P].rearrange("(j p) c -> p j c", p=P), writes=[bd])
            if n + 1 < len(its):
                xks[n + 1] = load_x(n + 1)
            tc = slice(th * TT, (th + 1) * TT)
            xk = xks.pop(n)
            py = PB[n_dn % 4]
            for jj in range(KH):
                hc = slice(jj * T + th * TT, jj * T + (th + 1) * TT)
                M.pe.op(lambda: nc.tensor.matmul(py[:], bd[:, jj, :], h[:, hc], start=(jj == 0), stop=(jj == KH - 1)),
                        reads=[bd, h], writes=[py])
            yo = W["yo"][n_dn % 2]
            M.dve.op(lambda: nc.vector.scalar_tensor_tensor(out=yo[:], in0=py[:], scalar=0.5, in1=xk[:], op0=ALU.mult, op1=ALU.add),
                     reads=[py, xk], writes=[yo])
            tok = M.sp.dma(outT[i * P:(i + 1) * P, tc], yo[:], reads=[yo])
            if half == 0:
                part[(i, th)] = tok
            else:
                toks.append(tok)
            n_dn += 1
    return toks


POOLW, DSAW, SBW, IDXH, IDXD = 1024, 1536, 1536, 16, 64
HD = 128


def col_offsets(D):
    sizes = [("up", POOLW), ("qb", DSAW), ("kb", DSAW), ("vb", DSAW), ("qi", IDXH * IDXD), ("ki", IDXD), ("wi", IDXH),
             ("qc", SBW), ("kc", SBW), ("vc", SBW)]
    off, o = {}, 0
    for n, s in sizes:
        off[n] = (o, s); o += s
    return off, o


def build_A(D, F, T, sizes=None, feat=("up", "qb", "kb", "qi", "ki", "qc", "kc"), tokm=("vb", "vc", "wi")):
    M = MK(); nc = M.nc
    KD = D // P
    off, INW = col_offsets(D)
    if sizes:
        off, INW = sizes
    xT = M.din("xT", [D, T])
    gains = M.din("gains", [P, 2 * KD])
    qkg = M.din("qkg", [P, 2])
    wg = M.din("wg", [D, F]); wu = M.din("wu", [D, F]); wd = M.din("wd", [F, D])
    w_in = M.din("w_in", [D, INW])
    hT = M.dout("hT", [D, T])
    outs = {}
    for n in ("up", "qb", "kb", "qi", "ki", "qc", "kc"):
        outs[n] = M.dout(n + "T", [off[n][1], T], BF16)
    for n in ("vb", "vc"):
        outs[n] = M.dout(n, [T, off[n][1]], BF16)
    outs["wi"] = M.dout("wi", [T, off["wi"][1]], F32)

    C = {}; load_consts(M, C)
    W = make_ffn_work2(M, D, F, T)
    NT = T // TT
    gs = M.sb("gains_sb", [P, 2 * KD], F32)
    M.sp.dma(gs[:], gains[:, :], writes=[gs])
    qk = M.sb("qkg_sb", [P, 2], F32)
    M.sp.dma(qk[:], qkg[:, :], writes=[qk])
    ev = [M.sb(f"a_ev{i}", [P, TT], BF16) for i in range(2)]
    qf = [M.sb(f"a_qf{i}", [P, TT], F32) for i in range(2)]
    evw = [M.sb(f"a_evw{i}", [P, 16], F32) for i in range(2)]
    PB = W["pb"]
    toks = []
    ftoks = ffn_full(M, C, W, xT, _view(gs, 0, KD), wg, wu, wd, hT, D, F, T)
    toks += ftoks
    M.sp._wait(ftoks)
    g_mix = _view(gs, KD, KD)
    xn = W["xn"]
    for th in range(NT):
        norm_tile(M, C, W, hT, g_mix, D, th * TT, xn, c0=th * TT)
    n_ev = 0
    n_w = 0
    for name in feat:
        o, sz = off[name]
        for c0 in range(0, sz, P):
            m = min(P, sz - c0)
            bw = W["wg"][n_w % 2]; n_w += 1
            M.pool.dma(bw[:, :, 0:m], w_in[:, o + c0:o + c0 + m].rearrange("(k p) f -> p k f", p=P), writes=[bw])
            for th in range(NT):
                tc = slice(th * TT, (th + 1) * TT)
                pp = PB[n_ev % 4]
                for k in range(KD):
                    M.pe.op(lambda: nc.tensor.matmul(pp[0:m, :], bw[:, k, 0:m], xn[:, k, tc], start=(k == 0), stop=(k == KD - 1)),
                            reads=[bw, xn], writes=[pp])
                e = ev[n_ev % 2]
                if name in ("qb", "kb"):
                    gi = 0 if name == "qb" else 1
                    f = qf[n_ev % 2]; sq = W["sq"][n_ev % 2]; ss = PB[4 + n_ev % 2]; rstd = W["rstd"]
                    M.dve.op(lambda: nc.vector.tensor_copy(out=f[:], in_=pp[:]), reads=[pp], writes=[f])
                    M.act.op(lambda: nc.scalar.activation(out=sq[:], in_=f[:], func=AF.Square), reads=[f], writes=[sq])
                    M.pe.op(lambda: nc.tensor.matmul(ss[:], C["ones_bf"][:], sq[:], start=True, stop=True),
                            reads=[sq, C["ones_bf"]], writes=[ss])
                    M.act.op(lambda: nc.scalar.activation(out=rstd[:], in_=ss[:], func=AF.Sqrt, bias=C["eps"][:], scale=1.0 / HD),
                             reads=[ss, C["eps"]], writes=[rstd])
                    M.dve.op(lambda: nc.vector.reciprocal(out=rstd[:], in_=rstd[:]), reads=[rstd], writes=[rstd])
                    M.dve.op(lambda: nc.vector.scalar_tensor_tensor(out=e[:], in0=f[:], scalar=qk[:, gi:gi + 1], in1=rstd[:],
                                                                      op0=ALU.mult, op1=ALU.mult),
                             reads=[f, qk, rstd], writes=[e])
                else:
                    M.act.op(lambda: nc.scalar.copy(out=e[0:m, :], in_=pp[0:m, :]), reads=[pp], writes=[e])
                toks.append(M.sp.dma(outs[name][c0:c0 + m, tc], e[0:m, :], reads=[e]))
                n_ev += 1
    h = W["h"]
    CW = 256
    nv = 0
    n_t = 0
    for name in tokm:
        o, sz = off[name]
        for c0 in range(0, sz, CW):
            m = min(CW, sz - c0)
            base = (nv % 2) * KD * CW
            wv = h.ap[:, base:base + KD * CW].rearrange("p (k c) -> p k c", k=KD)
            M.pool.dma(wv[:, :, 0:m], w_in[:, o + c0:o + c0 + m].rearrange("(k p) f -> p k f", p=P), writes=[h])
            for ts in range(T // P):
                pp = PB[n_t % 4]
                for k in range(KD):
                    M.pe.op(lambda: nc.tensor.matmul(pp[:, 0:m], xn[:, k, ts * P:(ts + 1) * P], wv[:, k, 0:m],
                                                     start=(k == 0), stop=(k == KD - 1)),
                            reads=[h, xn], writes=[pp])
                e = evw[n_t % 2] if name == "wi" else ev[n_t % 2]
                M.act.op(lambda: nc.scalar.copy(out=e[:, 0:m], in_=pp[:, 0:m]), reads=[pp], writes=[e])
                toks.append(M.sp.dma(outs[name][ts * P:(ts + 1) * P, c0:c0 + m], e[:, 0:m], reads=[e]))
                n_t += 1
            nv += 1
    return M.finish(toks)


class _view:
    def __init__(self, parent, o, n):
        self.parent, self.o, self.n = parent, o, n
        self.name = parent.name
    @property
    def writer(self): return self.parent.writer
    @writer.setter
    def writer(self, v): self.parent.writer = v
    @property
    def readers(self): return self.parent.readers
    @readers.setter
    def readers(self, v): self.parent.readers = v
    def __getitem__(self, idx):
        rows, cols = idx
        assert isinstance(cols, slice)
        return self.parent.ap[rows, self.o + cols.start:self.o + cols.stop]


WINS = (2, 4, 8, 16)
HALO = 15
NEG = -1.0e30


def build_B(D, F, NSLOT, NHD, NHS, TOPK, do_ffn=True):
    M = MK(); nc = M.nc
    T = NSLOT * P
    S = 8 * T
    KD = D // P
    assert D == POOLW + HD * (NHD + NHS) and NHD % 4 == 0 and NHS % 4 == 0
    SW = P + HALO
    hT = M.din("hT", [D, T])
    up_h = M.din("up_h", [POOLW, NSLOT * SW], BF16)
    invcnt = M.din("invcnt", [P, 4 * T])
    pool_w = M.din("pool_w", [4 * 256, 256])
    pscale = M.din("pscale", [P, 8])
    ident_d = M.din("ident", [P, P], BF16)
    trineg_d = M.din("trineg", [P, P], BF16)
    qbT = M.din("qbT", [NHD * HD, T], BF16)
    qiT = M.din("qiT", [1024, T], BF16)
    wi = M.din("wi", [T, 16])
    qcT = M.din("qcT", [NHS * HD, T], BF16)
    kbT_all = M.din("kbT_all", [NHD * HD, S], BF16)
    vb_all = M.din("vb_all", [S, NHD * HD], BF16)
    kiT_all = M.din("kiT_all", [64, S], BF16)
    kcT_all = M.din("kcT_all", [NHS * HD, S], BF16)
    vc_all = M.din("vc_all", [S, NHS * HD], BF16)
    biasraw = M.din("biasraw", [P, 9 * NHD * HD])
    biasfar = M.din("biasfar", [P, NHD * HD])
    sbmask_d = M.din("sbmask", [P, 8 * 512], BF16)
    idxneg_d = M.din("idxneg", [P, 8 * P])
    w_out = M.din("w_out", [D, D])
    gains2 = M.din("gains2", [P, KD])
    wg = M.din("wg", [D, F]); wu = M.din("wu", [D, F]); wd = M.din("wd", [F, D])
    outT = M.dout("outT", [D, T])
    mT_d = M.dscratch("mT_d", [D, T], BF16)
    h2T = M.dscratch("h2T", [D, T], F32)
    mtoks = []

    C = {}; load_consts(M, C)
    C["one"] = M.sb("onec", [P, 1], F32)
    M.dve.op(lambda: nc.vector.memset(C["one"][:], 1.0), writes=[C["one"]])
    C["onesneg"] = M.sb("onesneg", [P, P], BF16)
    M.dve.op(lambda: nc.vector.memset(C["onesneg"][:], -1.0), writes=[C["onesneg"]])
    ident = M.sb("ident_sb", [P, P], BF16)
    M.sp.dma(ident[:], ident_d[:, :], writes=[ident])
    trineg = M.sb("trineg_sb", [P, P], BF16)
    M.sp.dma(trineg[:], trineg_d[:, :], writes=[trineg])

    with M.phase():
        ub = [M.sb(f"p_ub{i}", [P, NSLOT * SW], BF16) for i in range(2)]
        sa = [M.sb(f"p_sa{i}", [P, NSLOT * SW], F32) for i in range(3)]
        icn = M.sb("p_icn", [P, 4 * T], F32)
        M.sp.dma(icn[:], invcnt[:, :], writes=[icn])
        psc = M.sb("p_psc", [P, 8], F32)
        M.sp.dma(psc[:], pscale[:, :], writes=[psc])
        dT = [M.sb(f"p_dT{k}", [P, T], BF16) for k in range(8)]
        tmp = [M.sb(f"p_tmp{i}", [P, T], F32) for i in range(2)]
        pw = [M.sb(f"p_pw{i}", [P, 2, 256], BF16) for i in range(2)]
        pps = [M.ps(f"p_ps{i}", [P, 512], F32) for i in range(2)]
        pev = [M.sb(f"p_ev{i}", [P, 512], BF16) for i in range(2)]
        v3 = lambda b: b.ap[:, :].rearrange("p (s w) -> p s w", w=SW)
        for k in range(8):
            g = k // 2; w = WINS[g]
            u = ub[k % 2]
            M.sp.dma(u[:], up_h[k * P:(k + 1) * P, :], writes=[u])
            uf = sa[0]
            M.act.op(lambda: nc.scalar.copy(out=uf[:], in_=u[:]), reads=[u], writes=[uf])
            cur, other = uf, 1
            sh = 1
            while sh < w:
                nxt = sa[other]
                c3, n3 = v3(cur), v3(nxt)
                M.dve.op(lambda: nc.vector.tensor_copy(out=n3[:, :, 0:sh], in_=c3[:, :, 0:sh]), reads=[cur], writes=[nxt])
                M.dve.op(lambda: nc.vector.tensor_tensor(out=n3[:, :, sh:SW], in0=c3[:, :, sh:SW], in1=c3[:, :, 0:SW - sh], op=ALU.add),
                         reads=[cur], writes=[nxt])
                cur = nxt
                other = 2 if other == 1 else 1
                sh *= 2
            c3 = v3(cur); u3 = v3(uf)
            t3 = tmp[k % 2]
            t3v = t3.ap[:, :].rearrange("p (s q) -> p s q", q=P)
            icv = icn.ap[:, g * T:(g + 1) * T].rearrange("p (s q) -> p s q", q=P)
            M.dve.op(lambda: nc.vector.tensor_tensor(out=t3v, in0=c3[:, :, HALO:SW], in1=icv, op=ALU.mult),
                     reads=[cur, icn], writes=[t3])
            dv = dT[k].ap[:, :].rearrange("p (s q) -> p s q", q=P)
            M.dve.op(lambda: nc.vector.tensor_tensor(out=dv, in0=t3v, in1=u3[:, :, HALO:SW], op=ALU.subtract),
                     reads=[t3, uf], writes=[dT[k]])
        n_e = 0
        for g in range(4):
            pwb = pw[g % 2]
            M.pool.dma(pwb[:], pool_w[g * 256:(g + 1) * 256, :].rearrange("(k p) f -> p k f", p=P), writes=[pwb])
            for co in range(2):
                for t0 in range(0, T, 512):
                    tn = min(512, T - t0)
                    pp = pps[n_e % 2]
                    for ci in range(2):
                        M.pe.op(lambda: nc.tensor.matmul(pp[:, 0:tn], pwb[:, ci, co * P:(co + 1) * P], dT[2 * g + ci][:, t0:t0 + tn],
                                                         start=(ci == 0), stop=(ci == 1)),
                                reads=[pwb, dT[2 * g + ci]], writes=[pp])
                    e = pev[n_e % 2]
                    ko = 2 * g + co
                    M.dve.op(lambda: nc.vector.tensor_scalar(out=e[:, 0:tn], in0=pp[:, 0:tn], scalar1=psc[:, ko:ko + 1], scalar2=None,
                                                              op0=ALU.mult),
                             reads=[pp, psc], writes=[e])
                    mtoks.append(M.sp.dma(mT_d[ko * P:(ko + 1) * P, t0:t0 + tn], e[:, 0:tn], reads=[e]))
                    n_e += 1

    with M.phase():
        NMAX = S
        ki2 = M.sb("a_ki2", [P, S], BF16)
        M.sp.dma(ki2[0:64, :], kiT_all[:, :], writes=[ki2])
        M.sp.dma(ki2[64:128, :], kiT_all[:, :], writes=[ki2])
        EB = M.sb("a_EB", [P, 9 * NHD * HD], BF16)
        EBf = M.sb("a_EBf", [P, NHD * HD], BF16)
        braw = M.sb("a_braw", [P, NHD * HD], F32)
        for r in range(9):
            M.sp.dma(braw[:], biasraw[:, r * NHD * HD:(r + 1) * NHD * HD], writes=[braw])
            M.act.op(lambda: nc.scalar.activation(out=EB[:, r * NHD * HD:(r + 1) * NHD * HD], in_=braw[:], func=AF.Exp),
                     reads=[braw], writes=[EB])
        M.sp.dma(braw[:], biasfar[:, :], writes=[braw])
        M.act.op(lambda: nc.scalar.activation(out=EBf[:], in_=braw[:], func=AF.Exp), reads=[braw], writes=[EBf])
        sbm = M.sb("a_sbm", [P, 8 * 512], BF16)
        M.sp.dma(sbm[:], sbmask_d[:, :], writes=[sbm])
        ineg = M.sb("a_ineg", [P, 8 * P], F32)
        M.sp.dma(ineg[:], idxneg_d[:, :], writes=[ineg])
        idx = M.sb("a_idx", [P, NMAX], F32)
        sel = M.sb("a_sel", [P, NMAX], BF16)
        selT = M.sb("a_selT", [P, NMAX], BF16)
        mx8 = M.sb("a_mx8", [P, 8], F32)
        qb_s = M.sb("a_qb", [P, NHD, P], BF16)
        qc_s = M.sb("a_qc", [P, NHS, P], BF16)
        qi_s = M.sb("a_qi", [P, 8, P], BF16)
        wi_s = M.sb("a_wi", [P, 16], F32)
        rl = [M.sb(f"a_rl{i}", [P, 512], F32) for i in range(2)]
        kt = [M.sb(f"a_kt{i}", [P, 4, 4 * P], BF16) for i in range(2)]
        vt = [M.sb(f"a_vt{i}", [P, 4, 4 * HD], BF16) for i in range(2)]
        pS = [M.sb(f"a_p{i}", [P, 512], F32) for i in range(3)]
        EM = [M.sb(f"a_EM{i}", [P, 512], BF16) for i in range(3)]
        pm = [M.sb(f"a_pm{i}", [P, 512], BF16) for i in range(3)]
        rinv = M.sb("a_rinv", [P, 512], F32)
        oev = [M.sb(f"a_oev{i}", [P, 512], BF16) for i in range(2)]
        eS = [M.sb(f"a_e{i}", [P, 512], F32) for i in range(2)]
        spS = [M.sb(f"a_sp{i}", [P, 512], F32) for i in range(2)]
        spb = [M.sb(f"a_spb{i}", [P, 512], BF16) for i in range(2)]
        t1 = [M.sb(f"a_t1{i}", [P, 512], F32) for i in range(2)]
        t2 = [M.sb(f"a_t2{i}", [P, 512], F32) for i in range(2)]
        aS = [M.sb(f"a_a{i}", [P, 512], BF16) for i in range(2)]
        carry = M.sb("a_carry", [P, 512], F32)
        PB = [M.ps(f"a_pb{i}", [P, 512], F32) for i in range(7)]
        PT = M.ps("a_pt", [P, 4 * P], BF16)
        sc_dsa = float(HD) ** -0.5
        for s in range(NSLOT):
            J = 8 * (s + 1)
            n = J * P
            ts = slice(s * P, (s + 1) * P)
            M.sp.dma(qb_s[:], qbT[:, ts].rearrange("(h p) t -> p h t", p=P), writes=[qb_s])
            M.sp.dma(qc_s[:], qcT[:, ts].rearrange("(h p) t -> p h t", p=P), writes=[qc_s])
            M.sp.dma(qi_s[:], qiT[:, ts].rearrange("(h p) t -> p h t", p=P), writes=[qi_s])
            M.sp.dma(wi_s[:], wi[ts, :], writes=[wi_s])
            for kc in range(n // 512):
                acc = idx.ap[:, kc * 512:(kc + 1) * 512]
                for h in range(16):
                    sc = PB[h % 2]
                    pb0 = (h % 2) * 64
                    M.pe.op(lambda: nc.tensor.matmul(sc[:], qi_s[pb0:pb0 + 64, h // 2, :], ki2[pb0:pb0 + 64, kc * 512:(kc + 1) * 512],
                                                     start=True, stop=True),
                            reads=[qi_s, ki2], writes=[sc])
                    r_ = rl[h % 2]
                    M.act.op(lambda: nc.scalar.activation(out=r_[:], in_=sc[:], func=AF.Relu), reads=[sc], writes=[r_])
                    if h == 0:
                        M.dve.op(lambda: nc.vector.tensor_scalar(out=acc, in0=r_[:], scalar1=wi_s[:, 0:1], scalar2=None, op0=ALU.mult),
                                 reads=[r_, wi_s], writes=[idx])
                    else:
                        M.dve.op(lambda: nc.vector.scalar_tensor_tensor(out=acc, in0=r_[:], scalar=wi_s[:, h:h + 1], in1=acc,
                                                                          op0=ALU.mult, op1=ALU.add),
                                 reads=[r_, wi_s, idx], writes=[idx])
            M.dve.op(lambda: nc.vector.tensor_tensor(out=idx.ap[:, n - 8 * P:n], in0=idx.ap[:, n - 8 * P:n], in1=ineg[:], op=ALU.add),
                     reads=[idx, ineg], writes=[idx])
            for _ in range(TOPK // 8):
                M.dve.op(lambda: nc.vector.max(out=mx8[:], in_=idx.ap[:, 0:n]), reads=[idx], writes=[mx8])
                M.dve.op(lambda: nc.vector.match_replace(out=idx.ap[:, 0:n], in_to_replace=mx8[:], in_values=idx.ap[:, 0:n], imm_value=NEG),
                         reads=[idx, mx8], writes=[idx])
            M.dve.op(lambda: nc.vector.tensor_single_scalar(out=sel.ap[:, 0:n], in_=idx.ap[:, 0:n], scalar=-1.0e29, op=ALU.is_le),
                     reads=[idx], writes=[sel])
            for j0 in range(0, J, 4):
                for jj in range(4):
                    j = j0 + jj
                    M.pe.op(lambda: nc.tensor.transpose(PT[:, jj * P:(jj + 1) * P], sel.ap[:, j * P:(j + 1) * P], ident[:]),
                            reads=[sel, ident], writes=[PT])
                M.act.op(lambda: nc.scalar.copy(out=selT.ap[:, j0 * P:(j0 + 4) * P], in_=PT[:]), reads=[PT], writes=[selT])
            for g in range(NHD // 4):
                o_ps, rs_ps = PB[6], PB[5]
                def D1(j, g=g):
                    it = j % 3
                    r = j - 8 * s
                    cj, jj = j // 4, j % 4
                    k_t, v_t = kt[cj % 2], vt[cj % 2]
                    if jj == 3:
                        M.sp.dma(k_t[:], kbT_all[g * 512:(g + 1) * 512, cj * 512:(cj + 1) * 512].rearrange("(h p) t -> p h t", p=P), writes=[k_t])
                        M.sp.dma(v_t[:], vb_all[cj * 512:(cj + 1) * 512, g * 512:(g + 1) * 512].rearrange("(j p) c -> p j c", p=P), writes=[v_t])
                    l_ps = PB[it]
                    for hh in range(4):
                        M.pe.op(lambda: nc.tensor.matmul(l_ps[:, hh * P:(hh + 1) * P], k_t[:, hh, jj * P:(jj + 1) * P], qb_s[:, 4 * g + hh, :], start=True, stop=True),
                                reads=[k_t, qb_s], writes=[l_ps])
                    p_ = pS[it]
                    M.act.op(lambda: nc.scalar.activation(out=p_[:], in_=l_ps[:], func=AF.Exp, scale=sc_dsa), reads=[l_ps], writes=[p_])
                    if r <= -2:
                        ebv = EBf.ap[:, g * 512:(g + 1) * 512]; ebb = EBf
                    else:
                        base = (r + 1) * NHD * HD + g * 512
                        ebv = EB.ap[:, base:base + 512]; ebb = EB
                    em = EM[it]
                    M.pool.op(lambda: nc.gpsimd.tensor_tensor(out=em.ap[:, :].rearrange("p (h q) -> p h q", q=P),
                                                              in0=ebv.rearrange("p (h q) -> p h q", q=P),
                                                              in1=selT.ap[:, j * P:(j + 1) * P].unsqueeze(1).to_broadcast([P, 4, P]),
                                                              op=ALU.mult),
                              reads=[ebb, selT], writes=[em])
                def D2(j, g=g):
                    it = j % 3
                    cj, jj = j // 4, j % 4
                    v_t = vt[cj % 2]
                    p_, em, pm_ = pS[it], EM[it], pm[it]
                    M.dve.op(lambda: nc.vector.tensor_tensor(out=pm_[:], in0=p_[:], in1=em[:], op=ALU.mult), reads=[p_, em], writes=[pm_])
                    for hh in range(4):
                        M.pe.op(lambda: nc.tensor.matmul(o_ps[:, hh * P:(hh + 1) * P], v_t[:, jj, hh * HD:(hh + 1) * HD], pm_[:, hh * P:(hh + 1) * P],
                                                         start=(j == J - 1 and hh == 0), stop=(j == 0 and hh == 3)),
                                reads=[v_t, pm_], writes=[o_ps])
                    M.pe.op(lambda: nc.tensor.matmul(rs_ps[:], C["ones_bf"][:], pm_[:], start=(j == J - 1), stop=(j == 0)),
                            reads=[pm_, C["ones_bf"]], writes=[rs_ps])
                order = list(range(J - 1, -1, -1))
                for n_, j in enumerate(order):
                    D1(j)
                    if n_ >= 2:
                        D2(order[n_ - 2])
                D2(order[-2])
                D2(order[-1])
                M.dve.op(lambda: nc.vector.reciprocal(out=rinv[:], in_=rs_ps[:]), reads=[rs_ps], writes=[rinv])
                oe = oev[g % 2]
                M.dve.op(lambda: nc.vector.tensor_tensor(out=oe[:], in0=o_ps[:], in1=rinv[:], op=ALU.mult), reads=[o_ps, rinv], writes=[oe])
                for hh in range(4):
                    row = POOLW + (4 * g + hh) * HD
                    mtoks.append(M.sp.dma(mT_d[row:row + HD, ts], oe[:, hh * P:(hh + 1) * P], reads=[oe]))
            for g in range(NHS // 4):
                o_ps = PB[6]
                M.dve.op(lambda: nc.vector.memset(carry[:], 0.0), writes=[carry])
                def S1(j, g=g):
                    it = j % 2
                    r = j - 8 * s
                    cj, jj = j // 4, j % 4
                    k_t, v_t = kt[cj % 2], vt[cj % 2]
                    if jj == 3:
                        M.sp.dma(k_t[:], kcT_all[g * 512:(g + 1) * 512, cj * 512:(cj + 1) * 512].rearrange("(h p) t -> p h t", p=P), writes=[k_t])
                        M.sp.dma(v_t[:], vc_all[cj * 512:(cj + 1) * 512, g * 512:(g + 1) * 512].rearrange("(j p) c -> p j c", p=P), writes=[v_t])
                    z_ps, tri_ps, cs_ps = PB[it], PB[2 + it], PB[4 + it]
                    for hh in range(4):
                        M.pe.op(lambda: nc.tensor.matmul(z_ps[:, hh * P:(hh + 1) * P], k_t[:, hh, jj * P:(jj + 1) * P], qc_s[:, 4 * g + hh, :], start=True, stop=True),
                                reads=[k_t, qc_s], writes=[z_ps])
                    e_, sp_, spb_ = eS[it], spS[it], spb[it]
                    M.act.op(lambda: nc.scalar.activation(out=e_[:], in_=z_ps[:], func=AF.Exp, scale=sc_dsa), reads=[z_ps], writes=[e_])
                    M.act.op(lambda: nc.scalar.activation(out=sp_[:], in_=e_[:], func=AF.Ln, bias=C["one"][:], scale=1.0),
                             reads=[e_, C["one"]], writes=[sp_])
                    if r >= 0:
                        M.pool.op(lambda: nc.gpsimd.tensor_tensor(out=spb_[:], in0=sp_[:], in1=sbm.ap[:, r * 512:(r + 1) * 512], op=ALU.mult),
                                  reads=[sp_, sbm], writes=[spb_])
                    else:
                        M.pool.op(lambda: nc.gpsimd.tensor_copy(out=spb_[:], in_=sp_[:]), reads=[sp_], writes=[spb_])
                    M.pe.op(lambda: nc.tensor.matmul(tri_ps[:], trineg[:], spb_[:], start=True, stop=True), reads=[trineg, spb_], writes=[tri_ps])
                    M.pe.op(lambda: nc.tensor.matmul(cs_ps[:], C["onesneg"][:], spb_[:], start=True, stop=True),
                            reads=[C["onesneg"], spb_], writes=[cs_ps])
                    t1_ = t1[it]
                    M.dve.op(lambda: nc.vector.scalar_tensor_tensor(out=t1_[:], in0=z_ps[:], scalar=sc_dsa, in1=sp_[:], op0=ALU.mult, op1=ALU.subtract),
                             reads=[z_ps, sp_], writes=[t1_])
                def S2(j, g=g):
                    it = j % 2
                    r = j - 8 * s
                    cj, jj = j // 4, j % 4
                    v_t = vt[cj % 2]
                    tri_ps, cs_ps = PB[2 + it], PB[4 + it]
                    t1_, t2_, a_ = t1[it], t2[it], aS[it]
                    M.dve.op(lambda: nc.vector.tensor_tensor(out=t2_[:], in0=tri_ps[:], in1=t1_[:], op=ALU.add), reads=[tri_ps, t1_], writes=[t2_])
                    M.dve.op(lambda: nc.vector.tensor_tensor(out=t2_[:], in0=t2_[:], in1=carry[:], op=ALU.add), reads=[t2_, carry], writes=[t2_])
                    M.act.op(lambda: nc.scalar.activation(out=a_[:], in_=t2_[:], func=AF.Exp), reads=[t2_], writes=[a_])
                    if r >= 0:
                        M.pool.op(lambda: nc.gpsimd.tensor_tensor(out=a_[:], in0=a_[:], in1=sbm.ap[:, r * 512:(r + 1) * 512], op=ALU.mult),
                                  reads=[a_, sbm], writes=[a_])
                    M.dve.op(lambda: nc.vector.tensor_tensor(out=carry[:], in0=cs_ps[:], in1=carry[:], op=ALU.add), reads=[cs_ps, carry], writes=[carry])
                    for hh in range(4):
                        M.pe.op(lambda: nc.tensor.matmul(o_ps[:, hh * P:(hh + 1) * P], v_t[:, jj, hh * HD:(hh + 1) * HD], a_[:, hh * P:(hh + 1) * P],
                                                         start=(j == J - 1 and hh == 0), stop=(j == 0 and hh == 3)),
                                reads=[v_t, a_], writes=[o_ps])
                order = list(range(J - 1, -1, -1))
                for n_, j in enumerate(order):
                    S1(j)
                    if n_ >= 1:
                        S2(order[n_ - 1])
                S2(order[-1])
                oe = oev[g % 2]
                M.act.op(lambda: nc.scalar.copy(out=oe[:], in_=o_ps[:]), reads=[o_ps], writes=[oe])
                for hh in range(4):
                    row = POOLW + NHD * HD + (4 * g + hh) * HD
                    mtoks.append(M.sp.dma(mT_d[row:row + HD, ts], oe[:, hh * P:(hh + 1) * P], reads=[oe]))

    M.sp._wait(mtoks)
    toks = []
    with M.phase():
        Tw = max(T, TT)
        W = make_ffn_work2(M, D, F, Tw)
        gs = M.sb("gains2_sb", [P, KD], F32)
        M.sp.dma(gs[:], gains2[:, :], writes=[gs])
        xn = W["xn"]
        M.sp.dma(xn[:, :, 0:T], mT_d[:, :].rearrange("(k p) t -> p k t", p=P), writes=[xn])
        htoks = []
        dst = h2T if do_ffn else outT
        its = [(i, t0) for i in range(KD) for t0 in range(0, T, TT)]
        def load_h(n):
            i, t0 = its[n]
            tn = min(TT, T - t0)
            xk = W["xk"][n % 3]
            M.sp.dma(xk[:, 0:tn], hT[i * P:(i + 1) * P, t0:t0 + tn], writes=[xk])
            return xk
        xks = {0: load_h(0)}
        for n, (i, t0) in enumerate(its):
            if t0 == 0:
                bw = W["wg"][i % 2]
                M.pool.dma(bw[:], w_out[:, i * P:(i + 1) * P].rearrange("(k p) f -> p k f", p=P), writes=[bw])
            if n + 1 < len(its):
                xks[n + 1] = load_h(n + 1)
            tn = min(TT, T - t0)
            xk = xks.pop(n)
            py = W["pb"][n % 4]
            for k in range(KD):
                M.pe.op(lambda: nc.tensor.matmul(py[:, 0:tn], bw[:, k, :], xn[:, k, t0:t0 + tn], start=(k == 0), stop=(k == KD - 1)),
                        reads=[bw, xn], writes=[py])
            yo = W["yo"][n % 2]
            M.dve.op(lambda: nc.vector.tensor_tensor(out=yo[:, 0:tn], in0=py[:, 0:tn], in1=xk[:, 0:tn], op=ALU.add),
                     reads=[py, xk], writes=[yo])
            htoks.append(M.sp.dma(dst[i * P:(i + 1) * P, t0:t0 + tn], yo[:, 0:tn], reads=[yo]))
        M.sp._wait(htoks)
        toks += htoks
        if do_ffn:
            toks += ffn_full(M, C, W, h2T, gs, wg, wu, wd, outT, D, F, T)
    return M.finish(toks)

BF = ml_dtypes.bfloat16
P = 128
NEG = -1.0e30


def rel_bucket_np(n):
    n = np.maximum(n, 0)
    nf = np.maximum(n, 1).astype(np.float32)
    large = 16 + (np.log(nf / np.float32(16)) / np.float32(math.log(128 / 16)) * np.float32(16)).astype(np.int32)
    large = np.minimum(large, 31)
    return np.where(n < 16, n, large)


def core_tokens(c, NSLOT):
    return np.concatenate([np.arange((8 * s + c) * P, (8 * s + c + 1) * P) for s in range(NSLOT)])


def core_consts(c, NSLOT, rel_bias, NHD):
    k = np.arange(P)[:, None]; q = np.arange(P)[None, :]
    braw = np.zeros((P, 9, NHD, P), np.float32)
    for ri, r in enumerate(range(-1, 8)):
        dist = 128 * (c - r) + q - k
        b = rel_bias[rel_bucket_np(dist)]
        b = np.where((dist >= 0)[:, :, None], b, np.float32(-30000.0))
        braw[:, ri] = np.transpose(b, (0, 2, 1))[:, :NHD]
    bfar = np.broadcast_to(rel_bias[31][None, :NHD, None], (P, NHD, P)).astype(np.float32)
    sbm = np.zeros((P, 8, 4, P), np.float32)
    ineg = np.zeros((P, 8, P), np.float32)
    for r in range(8):
        dist = 128 * (c - r) + q - k
        sbm[:, r] = (dist > 0)[:, None, :]
        ineg[:, r] = np.where(dist.T >= 0, 0.0, NEG)
    T = NSLOT * P
    gt = core_tokens(c, NSLOT)
    inv = np.stack([1.0 / np.minimum(gt + 1, w) for w in (2, 4, 8, 16)]).astype(np.float32)
    inv = np.broadcast_to(inv.reshape(1, 4 * T), (P, 4 * T))
    return {"biasraw": np.ascontiguousarray(braw.reshape(P, -1)), "biasfar": np.ascontiguousarray(bfar.reshape(P, -1)),
            "sbmask": np.ascontiguousarray(sbm.reshape(P, -1)).astype(BF), "idxneg": np.ascontiguousarray(ineg.reshape(P, -1)),
            "invcnt": np.ascontiguousarray(inv)}


def shared_consts():
    j = np.arange(P)[:, None]; s = np.arange(P)[None, :]
    return {"ident": np.eye(P, dtype=np.float32).astype(BF), "trineg": np.where(j > s, -1.0, 0.0).astype(np.float32).astype(BF)}


def up_halo(upT_glob, c, NSLOT):
    out = np.zeros((upT_glob.shape[0], NSLOT, P + 15), upT_glob.dtype)
    for s in range(NSLOT):
        g0 = (8 * s + c) * P
        lo = max(0, g0 - 15)
        out[:, s, 15 - (g0 - lo):] = upT_glob[:, lo:g0 + P]
    return np.ascontiguousarray(out.reshape(upT_glob.shape[0], -1))


def pk(g, D):
    return np.ascontiguousarray(np.asarray(g, np.float32).reshape(D // P, P).T)


D_MODEL, SEQ, DEPTH, D_FF = 4096, 8192, 4, 6144
NCORE, NSLOT_FULL, NHD_FULL, NHS_FULL, TOPK_FULL = 8, 8, 12, 12, 256


def kernel(x, positions, rel_bias, ffn1_norm, ffn1_gate, ffn1_up, ffn1_down, mix_norm, w_in, pool_w, pool_scale,
           q_norm, k_norm, w_out, ffn2_norm, ffn2_gate, ffn2_up, ffn2_down):
    f32 = np.float32
    D, S, T = D_MODEL, SEQ, NSLOT_FULL * P
    x = np.asarray(x, f32)
    rel_bias = np.asarray(rel_bias, f32)
    ncA = build_A(D, D_FF, T)
    ncB = build_B(D, D_FF, NSLOT_FULL, NHD_FULL, NHS_FULL, TOPK_FULL, do_ffn=True)
    toks = [core_tokens(c, NSLOT_FULL) for c in range(NCORE)]
    consts = [core_consts(c, NSLOT_FULL, rel_bias, NHD_FULL) for c in range(NCORE)]
    sh = shared_consts()
    xT = [np.ascontiguousarray(x[0, toks[c], :].T) for c in range(NCORE)]
    cores = list(range(NCORE))
    for i in range(DEPTH):
        gains = np.concatenate([pk(ffn1_norm[i], D), pk(mix_norm[i], D)], 1)
        qkg = np.ascontiguousarray(np.stack([np.asarray(q_norm[i], f32), np.asarray(k_norm[i], f32)], 1))
        wA = {"gains": gains, "qkg": qkg, "wg": np.asarray(ffn1_gate[i], f32), "wu": np.asarray(ffn1_up[i], f32),
              "wd": np.asarray(ffn1_down[i], f32), "w_in": np.asarray(w_in[i], f32)}
        _t = time.time()
        ra = run_bass_kernel_spmd(ncA, [dict(wA, xT=xT[c]) for c in cores], core_ids=cores).results
        print(f'[kernel] layer {i} A {time.time() - _t:.1f}s', flush=True)
        def gather_T(name):
            out = np.empty((ra[0][name].shape[0], S), ra[0][name].dtype)
            for c in cores:
                out[:, toks[c]] = ra[c][name]
            return out
        def gather_R(name):
            out = np.empty((S, ra[0][name].shape[1]), ra[0][name].dtype)
            for c in cores:
                out[toks[c], :] = ra[c][name]
            return out
        upT_g = gather_T("upT")
        kv = {"kbT_all": gather_T("kbT"), "vb_all": gather_R("vb"), "kiT_all": gather_T("kiT"),
              "kcT_all": gather_T("kcT"), "vc_all": gather_R("vc")}
        wB = {"pool_w": np.ascontiguousarray(np.asarray(pool_w[i], f32).reshape(1024, 256)), "pscale": pk(pool_scale[i], 1024),
              "w_out": np.asarray(w_out[i], f32), "gains2": pk(ffn2_norm[i], D), "wg": np.asarray(ffn2_gate[i], f32),
              "wu": np.asarray(ffn2_up[i], f32), "wd": np.asarray(ffn2_down[i], f32)}
        wB.update(sh); wB.update(kv)
        in_b = []
        for c in cores:
            d = dict(wB); d.update(consts[c])
            d.update({"hT": ra[c]["hT"], "up_h": up_halo(upT_g, c, NSLOT_FULL), "qbT": ra[c]["qbT"], "qiT": ra[c]["qiT"],
                      "wi": ra[c]["wi"], "qcT": ra[c]["qcT"]})
            in_b.append(d)
        del ra
        _t = time.time()
        rb = run_bass_kernel_spmd(ncB, in_b, core_ids=cores).results
        print(f'[kernel] layer {i} B {time.time() - _t:.1f}s', flush=True)
        xT = [rb[c]["outT"] for c in cores]
        del rb, in_b
    out = np.empty((1, S, D), f32)
    for c in cores:
        out[0, toks[c], :] = xT[c].T
    return out
```
